# Optimizing a Trainium2 kernel written in Bass

```python
import jax, jax.numpy as jnp
from jax import lax
import numpy as np

D_MODEL = 1024
BATCH = 8
SEQ = 4096
DEPTH = 1

N_Q_HEADS = 8
N_KV_HEADS = 2
HEAD_DIM = 64
WINDOW = 128
GQA_GROUP = N_Q_HEADS // N_KV_HEADS
M_HEADS = 4
M_QK_DIM = 64
M_V_DIM = 128
CONV_WIDTH = 4
CHUNK = 64
N_GROUPS = 4
EXPERTS_PER_GROUP = 8
N_EXPERTS = N_GROUPS * EXPERTS_PER_GROUP
TOP_K = 2
D_EXPERT = 512
EXPERT_BLOCK = 256
EPS = 1e-6

ATT_Q_WIDTH = N_Q_HEADS * HEAD_DIM
ATT_KV_WIDTH = N_KV_HEADS * HEAD_DIM
M_QK_WIDTH = M_HEADS * M_QK_DIM
M_V_WIDTH = M_HEADS * M_V_DIM
IN_SIZES = (ATT_Q_WIDTH, ATT_KV_WIDTH, ATT_KV_WIDTH, M_QK_WIDTH, M_QK_WIDTH, M_V_WIDTH, M_V_WIDTH, M_HEADS, M_HEADS, D_MODEL, D_MODEL)
IN_WIDTH = sum(IN_SIZES)

kernel_name = 'hybrid_swa_mlstm_hmoe_block'


def rms_norm(x, w):
    xf = x.astype(jnp.float32)
    y = xf * lax.rsqrt(jnp.mean(xf * xf, axis=-1, keepdims=True) + EPS)
    return (y * w.astype(jnp.float32)).astype(x.dtype)


def causal_conv(u, w, b):
    c = u.shape[-1]
    y = lax.conv_general_dilated(u, w[:, None, :].astype(u.dtype), window_strides=(1,),
                                 padding=[(CONV_WIDTH - 1, 0)],
                                 dimension_numbers=('NWC', 'WIO', 'NWC'),
                                 feature_group_count=c)
    return y + b.astype(u.dtype)


def sliding_window_attention(q, k, v, sinks):
    B, S, _ = q.shape
    nb = S // WINDOW
    f32 = jnp.float32
    qb = q.astype(f32).reshape(B, nb, WINDOW, N_KV_HEADS, GQA_GROUP, HEAD_DIM)

    def band_keys(t):
        t = t.astype(f32).reshape(B, S, N_KV_HEADS, HEAD_DIM)
        t = jnp.concatenate([jnp.zeros((B, WINDOW, N_KV_HEADS, HEAD_DIM), f32), t], axis=1)
        t = t.reshape(B, nb + 1, WINDOW, N_KV_HEADS, HEAD_DIM)
        return jnp.concatenate([t[:, :-1], t[:, 1:]], axis=2)

    kw, vw = band_keys(k), band_keys(v)
    s = jnp.einsum('bnqkgd,bnskd->bnkgqs', qb, kw) * (HEAD_DIM ** -0.5)
    qi = jnp.arange(WINDOW)[:, None]
    kj = jnp.arange(2 * WINDOW)[None, :]
    band = (kj > qi) & (kj <= qi + WINDOW)
    valid = band[None] & ((jnp.arange(nb)[:, None, None] > 0) | (kj[None] >= WINDOW))
    s = jnp.where(valid[None, :, None, None], s, -jnp.inf)
    sink = jnp.broadcast_to(sinks.astype(f32).reshape(1, 1, N_KV_HEADS, GQA_GROUP, 1, 1), s.shape[:-1] + (1,))
    p = jax.nn.softmax(jnp.concatenate([s, sink], axis=-1), axis=-1)[..., :-1]
    o = jnp.einsum('bnkgqs,bnskd->bnqkgd', p, vw)
    return o.reshape(B, S, ATT_Q_WIDTH).astype(q.dtype)


def mlstm_chunkwise(q, k, v, ig, lf):
    B, S, H, DK = q.shape
    DV = v.shape[-1]
    nc = S // CHUNK

    def chunks(t):
        return t.reshape(B, nc, CHUNK, H, -1).transpose(1, 0, 3, 2, 4)

    def gate_chunks(t):
        return t.reshape(B, nc, CHUNK, H).transpose(1, 0, 3, 2)

    causal = jnp.tril(jnp.ones((CHUNK, CHUNK), dtype=bool))

    def step(carry, inp):
        C, n, m = carry
        qb, kb, vb, ib, fb = inp
        b = jnp.cumsum(fb, axis=-1)
        a = b + m[..., None]
        dmat = jnp.where(causal, b[..., :, None] - b[..., None, :] + ib[..., None, :], -jnp.inf)
        mt = jnp.maximum(a, jnp.max(dmat, axis=-1))
        wq = jnp.exp(dmat - mt[..., None])
        wa = jnp.exp(a - mt)
        sw = jnp.einsum('bhtd,bhsd->bhts', qb, kb) * wq
        num = wa[..., None] * jnp.einsum('bhtd,bhdv->bhtv', qb, C) + jnp.einsum('bhts,bhsv->bhtv', sw, vb)
        den = wa * jnp.einsum('bhtd,bhd->bht', qb, n) + jnp.sum(sw, axis=-1)
        hb = num / jnp.maximum(jnp.abs(den), jnp.exp(-mt))[..., None]
        m_new = mt[..., -1]
        wc = jnp.exp(b[..., -1] + m - m_new)
        ws = jnp.exp(b[..., -1:] - b + ib - m_new[..., None])
        C_new = wc[..., None, None] * C + jnp.einsum('bhs,bhsd,bhsv->bhdv', ws, kb, vb)
        n_new = wc[..., None] * n + jnp.einsum('bhs,bhsd->bhd', ws, kb)
        return (C_new, n_new, m_new), hb

    f32 = jnp.float32
    init = (jnp.zeros((B, H, DK, DV), f32), jnp.zeros((B, H, DK), f32), jnp.zeros((B, H), f32))
    _, hs = lax.scan(step, init, (chunks(q), chunks(k), chunks(v), gate_chunks(ig), gate_chunks(lf)))
    return hs.transpose(1, 0, 3, 2, 4).reshape(B, S, H, DV)


def hierarchical_moe(h, w_group, b_group, w_router, b_router, w_gate, w_up, w_down):
    B, S, D = h.shape
    T = B * S
    f32 = jnp.float32
    xt = h.reshape(T, D)
    gprob = jax.nn.softmax((xt @ w_group).astype(f32) + b_group.astype(f32), axis=-1)
    gp, gi = lax.top_k(gprob, 1)
    elog = ((xt @ w_router).astype(f32) + b_router.astype(f32)).reshape(T, N_GROUPS, EXPERTS_PER_GROUP)
    elog_g = jnp.take_along_axis(elog, gi[:, :, None], axis=1)[:, 0]
    ev, ej = lax.top_k(elog_g, TOP_K)
    ew = jax.nn.softmax(ev, axis=-1) * gp
    eid = gi * EXPERTS_PER_GROUP + ej

    A = T * TOP_K
    flat_e = eid.reshape(A)
    flat_tok = jnp.repeat(jnp.arange(T, dtype=jnp.int32), TOP_K)
    flat_w = ew.reshape(A)
    order = jnp.argsort(flat_e, stable=True)
    se, stok, sw = flat_e[order], flat_tok[order], flat_w[order]
    counts = jnp.bincount(flat_e, length=N_EXPERTS)
    starts = jnp.cumsum(counts) - counts
    pcounts = ((counts + EXPERT_BLOCK - 1) // EXPERT_BLOCK) * EXPERT_BLOCK
    pends = jnp.cumsum(pcounts)
    pstarts = pends - pcounts
    dest = pstarts[se] + jnp.arange(A, dtype=jnp.int32) - starts[se]
    nblk = (A + EXPERT_BLOCK - 1) // EXPERT_BLOCK + N_EXPERTS
    P = nblk * EXPERT_BLOCK
    slot_tok = jnp.full((P,), T, dtype=jnp.int32).at[dest].set(stok)
    slot_w = jnp.zeros((P,), f32).at[dest].set(sw)
    blk_e = jnp.minimum(jnp.searchsorted(pends, jnp.arange(nblk, dtype=jnp.int32) * EXPERT_BLOCK, side='right'), N_EXPERTS - 1)
    xpad = jnp.concatenate([xt, jnp.zeros((1, D), xt.dtype)], axis=0)
    xs = xpad[slot_tok].reshape(nblk, EXPERT_BLOCK, D)

    def expert_block(args):
        xb, e = args
        return (jax.nn.silu(xb @ w_gate[e]) * (xb @ w_up[e])) @ w_down[e]

    ys = lax.map(expert_block, (xs, blk_e)).reshape(P, D)
    y = jnp.zeros((T + 1, D), ys.dtype).at[slot_tok].add(ys * slot_w[:, None].astype(ys.dtype))[:T]
    return y.reshape(B, S, D)


def setup_inputs(seed: int = 0) -> dict:
    key = jax.random.key(seed)
    ks = jax.random.split(key, 24)
    f32 = jnp.float32

    def nrm(k, shape, scale):
        return jax.random.normal(k, shape, f32) * scale

    L = DEPTH
    return {
        'x': nrm(ks[0], (BATCH, SEQ, D_MODEL), 1.0),
        'norm_mix_w': 1.0 + nrm(ks[1], (L, D_MODEL), 0.02),
        'w_in': nrm(ks[2], (L, D_MODEL, IN_WIDTH), D_MODEL ** -0.5),
        'conv_w': nrm(ks[3], (L, CONV_WIDTH, 2 * M_QK_WIDTH), CONV_WIDTH ** -0.5),
        'conv_b': nrm(ks[4], (L, 2 * M_QK_WIDTH), 0.02),
        'b_igate': nrm(ks[5], (L, M_HEADS), 0.1),
        'b_fgate': jnp.linspace(3.0, 6.0, M_HEADS, dtype=f32)[None] + nrm(ks[6], (L, M_HEADS), 0.1),
        'attn_sinks': nrm(ks[7], (L, N_Q_HEADS), 0.5),
        'mlstm_norm_w': 1.0 + nrm(ks[8], (L, M_V_WIDTH), 0.02),
        'w_attn_o': nrm(ks[9], (L, ATT_Q_WIDTH, D_MODEL), ATT_Q_WIDTH ** -0.5),
        'w_mlstm_o': nrm(ks[10], (L, M_V_WIDTH, D_MODEL), M_V_WIDTH ** -0.5),
        'w_out': nrm(ks[11], (L, D_MODEL, D_MODEL), D_MODEL ** -0.5),
        'norm_ffn_w': 1.0 + nrm(ks[12], (L, D_MODEL), 0.02),
        'w_group': nrm(ks[13], (L, D_MODEL, N_GROUPS), D_MODEL ** -0.5),
        'b_group': nrm(ks[14], (L, N_GROUPS), 0.01),
        'w_router': nrm(ks[15], (L, D_MODEL, N_EXPERTS), D_MODEL ** -0.5),
        'b_router': nrm(ks[16], (L, N_EXPERTS), 0.01),
        'w_gate': nrm(ks[17], (L, N_EXPERTS, D_MODEL, D_EXPERT), D_MODEL ** -0.5),
        'w_up': nrm(ks[18], (L, N_EXPERTS, D_MODEL, D_EXPERT), D_MODEL ** -0.5),
        'w_down': nrm(ks[19], (L, N_EXPERTS, D_EXPERT, D_MODEL), D_EXPERT ** -0.5),
        'norm_final_w': 1.0 + nrm(ks[20], (D_MODEL,), 0.02),
    }


def reference(x, norm_mix_w, w_in, conv_w, conv_b, b_igate, b_fgate, attn_sinks, mlstm_norm_w,
              w_attn_o, w_mlstm_o, w_out, norm_ffn_w, w_group, b_group, w_router, b_router,
              w_gate, w_up, w_down, norm_final_w):
    B, S, _ = x.shape
    f32 = jnp.float32
    splits = np.cumsum(np.array(IN_SIZES))[:-1].tolist()
    for l in range(DEPTH):
        h = rms_norm(x, norm_mix_w[l])
        proj = h @ w_in[l]
        aq, ak, av, mq, mk, mv, mo, mi, mf, ga, gb = jnp.split(proj, splits, axis=-1)

        ya = sliding_window_attention(aq, ak, av, attn_sinks[l])

        qk = jax.nn.silu(causal_conv(jnp.concatenate([mq, mk], axis=-1), conv_w[l], conv_b[l]))
        mq, mk = jnp.split(qk, 2, axis=-1)
        q_m = mq.astype(f32).reshape(B, S, M_HEADS, M_QK_DIM)
        k_m = mk.astype(f32).reshape(B, S, M_HEADS, M_QK_DIM) * (M_QK_DIM ** -0.5)
        v_m = mv.astype(f32).reshape(B, S, M_HEADS, M_V_DIM)
        ig = mi.astype(f32) + b_igate[l].astype(f32)
        lf = jax.nn.log_sigmoid(mf.astype(f32) + b_fgate[l].astype(f32))
        hm = mlstm_chunkwise(q_m, k_m, v_m, ig, lf)
        hm = hm * lax.rsqrt(jnp.mean(hm * hm, axis=-1, keepdims=True) + EPS)
        hm = hm.reshape(B, S, M_V_WIDTH) * mlstm_norm_w[l].astype(f32)
        ym = (jax.nn.sigmoid(mo.astype(f32)) * hm).astype(x.dtype)

        mix = jax.nn.sigmoid(ga) * (ya @ w_attn_o[l]) + jax.nn.sigmoid(gb) * (ym @ w_mlstm_o[l])
        x = x + mix @ w_out[l]

        h = rms_norm(x, norm_ffn_w[l])
        x = x + hierarchical_moe(h, w_group[l], b_group[l], w_router[l], b_router[l],
                                 w_gate[l], w_up[l], w_down[l])
    return rms_norm(x, norm_final_w)
```

```python
import os
import numpy as np
import ml_dtypes
from contextlib import ExitStack
import concourse.bass as bass
import concourse.mybir as mybir
from concourse.bass_utils import run_bass_kernel_spmd

F32 = mybir.dt.float32
BF16 = mybir.dt.bfloat16
I32 = mybir.dt.int32
ALU = mybir.AluOpType
AF = mybir.ActivationFunctionType
AX = mybir.AxisListType

T = 4096
D = 1024
NT = 32
SW = 256
NS = int(os.environ.get('K_NS', T // SW))
BPS = SW // 128
CAP = 384
NBLK = CAP // 128
NE = 32
INW = 4360
EPS = 1e-6
LN32 = 3.4657359027997265
LN2 = 0.6931471805599453

C_AQ, C_AK, C_AV, C_MQ, C_MK, C_MV, C_MO, C_MI, C_GA, C_GB = 0, 512, 640, 768, 1024, 1280, 1792, 2304, 2312, 3336


class Sched:
    ENG = ("pe", "act", "dve", "pool", "sp")
    N_DMA_SEM = 24
    PSUM_KEYS = frozenset(("T0", "A0", "A1", "SC", "PV0", "PV1", "U0", "U1"))

    def __init__(self, nc, es):
        self.nc = nc
        self.q = {k: [] for k in self.ENG}
        self.sem = {k: es.enter_context(nc.semaphore("s_" + k)) for k in self.ENG}
        self.cnt = {k: 0 for k in self.ENG}
        self.dsem = [es.enter_context(nc.semaphore("s_dma%d" % i)) for i in range(self.N_DMA_SEM)]
        self.dcnt = [0] * self.N_DMA_SEM
        self.dpool = {"sp": list(range(0, 14)), "pool": list(range(14, self.N_DMA_SEM))}
        self.dnext = {"sp": 0, "pool": 0}
        self.waited = {k: {} for k in self.ENG}
        self.last_w = {}
        self.readers = {}

    def _semh(self, key):
        return self.sem[key] if isinstance(key, str) else self.dsem[key]

    def _wait(self, engine, tok):
        key, val = tok
        if key == engine and engine == "pe":
            return
        if self.waited[engine].get(key, 0) >= val:
            return
        self.waited[engine][key] = val
        h = self._semh(key)
        self.q[engine].append(lambda e, h=h, val=val: e.wait_ge(h, val))

    def _deps(self, engine, reads, writes):
        best = {}

        def add(d):
            for k, v in d.items():
                if best.get(k, 0) < v:
                    best[k] = v
        for r in reads:
            add(self.last_w.get(r, {}))
        for w in writes:
            add(self.last_w.get(w, {}))
            add(self.readers.get(w, {}))
        for k, v in best.items():
            self._wait(engine, (k, v))

    def _commit(self, tok, reads, writes):
        k, v = tok
        for r in reads:
            d = self.readers.setdefault(r, {})
            if d.get(k, 0) < v:
                d[k] = v
        for w in writes:
            d = self.last_w.setdefault(w, {})
            if d.get(k, 0) < v:
                d[k] = v
            self.readers[w] = {}

    nops = 0
    maxops = int(os.environ.get("K_MAXOPS", 10 ** 9))

    def op(self, engine, fn, reads=(), writes=()):
        self.nops += 1
        if self.nops > self.maxops:
            return None
        pr = [r for r in reads if r in self.PSUM_KEYS]
        if pr:
            reads = [r for r in reads if r not in self.PSUM_KEYS]
            writes = list(writes) + pr
        self._deps(engine, reads, writes)
        self.cnt[engine] += 1
        tok = (engine, self.cnt[engine])
        h = self.sem[engine]
        self.q[engine].append(lambda e, fn=fn, h=h: fn(e).then_inc(h, 1))
        self._commit(tok, reads, writes)
        return tok

    def dma(self, queue, fn, reads=(), writes=()):
        self.nops += 1
        if self.nops > self.maxops:
            return None
        self._deps(queue, reads, writes)
        pl = self.dpool[queue]
        i = pl[self.dnext[queue]]
        self.dnext[queue] = (self.dnext[queue] + 1) % len(pl)
        if self.dcnt[i] > 0:
            self._wait(queue, (i, self.dcnt[i]))
        self.dcnt[i] += 16
        tok = (i, self.dcnt[i])
        h = self.dsem[i]
        self.q[queue].append(lambda e, fn=fn, h=h: fn(e).then_inc(h, 16))
        self._commit(tok, reads, writes)
        return tok

    def barrier(self):
        for e in self.ENG:
            for f in self.ENG:
                if f != e and self.cnt[f] > 0:
                    self._wait(e, (f, self.cnt[f]))
            for i in range(self.N_DMA_SEM):
                if self.dcnt[i] > 0:
                    self._wait(e, (i, self.dcnt[i]))

    def emit(self):
        eng = {"pe": "tensor", "act": "scalar", "dve": "vector", "pool": "gpsimd", "sp": "sync"}
        with self.nc.Block() as block:
            for k in self.ENG:
                def body(e, k=k):
                    for f in self.q[k]:
                        f(e)
                getattr(block, eng[k])(body)


def build_program(debug=False, stop_after_phase1=False):
    nc = bass.Bass("TRN2", target_bir_lowering=False)

    def din(name, shape, dt=F32):
        return nc.dram_tensor(name, list(shape), dt, kind="ExternalInput").ap()

    scratch_kind = "ExternalOutput" if debug else "Internal"

    def dscr(name, shape, dt):
        return nc.dram_tensor(name, list(shape), dt, kind=scratch_kind).ap()

    x_d = din("x", [T, D])
    w_in_d = din("w_in", [D, INW])
    wgab_d = din("w_gab", [16, 128, 8, 128])
    wao_d = din("w_attn_o", [512, D])
    wmo_d = din("w_mlstm_o", [512, D])
    wout_d = din("w_out", [D, D])
    wr_d = din("w_rt", [D, 36])
    wg_d = din("w_gate", [NE, D, 512])
    wu_d = din("w_up", [NE, D, 512])
    wd_d = din("w_down", [NE, 512, D])
    nmw_d = din("norm_mix_w", [1, D])
    nfw_d = din("norm_ffn_w", [1, D])
    nlw_d = din("norm_final_w", [1, D])
    mnw_d = din("mlstm_norm_w", [1, 512])
    cw_d = din("conv_wt", [128, 4, 4])
    cb_d = din("conv_bt", [128, 4])
    bg_d = din("b_gates", [1, 8])
    sk_d = din("attn_sinks", [1, 8])
    brt_d = din("b_rt", [1, 36])
    identb_d = din("ident_bf", [128, 128], BF16)
    identf_d = din("ident_f", [128, 128])
    maskb_d = din("maskb", [128, 3, 128], BF16)
    cmask8_d = din("cmask8", [128, 128])
    trif_d = din("tri_f", [128, 128])
    onesf_d = din("ones_f", [128, 128])
    trisb_d = din("tris_bf", [128, 128], BF16)
    onesb_d = din("ones_bf", [128, 128], BF16)
    eoff_d = din("eoff", [1, NE])
    out_d = nc.dram_tensor("out", [T, D], F32, kind="ExternalOutput").ap()

    x2_d = dscr("x2_s", [T, D], F32)
    h2_d = dscr("h2_s", [T, D], BF16)
    xs_d = dscr("xs_s", [NE * CAP, D], BF16)
    ys_d = dscr("ys_s", [NE * CAP, D], F32)
    if debug:
        lg_d = nc.dram_tensor("lg_s", [128, NT, 36], F32, kind="ExternalOutput").ap()
        dst_d = nc.dram_tensor("dst_s", [128, 2, NT], I32, kind="ExternalOutput").ap()
        wts_d = nc.dram_tensor("wts_s", [128, 2, NT], F32, kind="ExternalOutput").ap()
        ya_dbg = nc.dram_tensor("ya_s", [T, 512], BF16, kind="ExternalOutput").ap()
        ym_dbg = nc.dram_tensor("ym_s", [T, 512], BF16, kind="ExternalOutput").ap()

    with ExitStack() as es:
        def sb(stack, name, shape, dt):
            return stack.enter_context(nc.sbuf_tensor("sb_" + name, list(shape), dt))

        def ps(name, shape, dt):
            return es.enter_context(nc.psum_tensor("ps_" + name, list(shape), dt))

        S = Sched(nc, es)

        T0 = ps("T0", [128, 8, 128], BF16)
        A0 = ps("A0", [128, 512], F32)
        A1 = ps("A1", [128, 512], F32)
        SC = ps("SC", [128, 512], F32)
        SC3 = SC[:].rearrange("p (a q) -> p a q", a=4)
        PV0 = ps("PV0", [128, 512], F32)
        PV1 = ps("PV1", [128, 512], F32)
        U0 = ps("U0", [128, 512], F32)
        U1 = ps("U1", [128, 512], F32)
        PV = [PV0, PV1]
        UU = [U0, U1]
        AA = [A0, A1]
        _bk = {"U0": (U0, "U0"), "PV1": (PV1, "PV1"), "SC": (SC, "SC"), "A0": (A0, "A0"), "PV0": (PV0, "PV0"),
               "A1": (A1, "A1"), "U1": (U1, "U1")}
        BANKS_AD = [_bk[k_] for k_ in os.environ.get("K_BANKS", "U0,PV1").split(",")]
        BANKS_C = [(A0, "A0"), (A1, "A1"), (SC, "SC"), (PV0, "PV0"), (PV1, "PV1"), (U0, "U0"), (U1, "U1")]
        bank_i = [0, 0]

        def nextbank():
            b_ = BANKS_AD[bank_i[0] % len(BANKS_AD)]
            bank_i[0] += 1
            return b_

        def nextbankC():
            b_ = BANKS_C[bank_i[1] % len(BANKS_C)]
            bank_i[1] += 1
            return b_

        identb = sb(es, "identb", [128, 128], BF16)
        identf = sb(es, "identf", [128, 128], F32)
        onesb = sb(es, "onesb", [128, 128], BF16)
        trisb = sb(es, "trisb", [128, 128], BF16)
        epsb = sb(es, "epsb", [128, 1], F32)
        lg_all = sb(es, "lg_all", [128, NT, 36], F32)

        def ld(dst, src, key, q="sp"):
            S.dma(q, lambda e: e.dma_start(out=dst, in_=src), writes=[key])

        ld(identb[:], identb_d, "identb")
        ld(identf[:], identf_d, "identf")
        ld(onesb[:], onesb_d, "onesb")
        ld(trisb[:], trisb_d, "trisb")
        S.op("pool", lambda e: e.memset(epsb[:], EPS), writes=["epsb"])
        lnb = sb(es, "lnb", [128, 1], F32)
        S.op("pool", lambda e: e.memset(lnb[:], -LN32), writes=["lnb"])
        neghalf = sb(es, "neghalf", [128, 4], F32)
        S.op("pool", lambda e: e.memset(neghalf[:], -0.5), writes=["neghalf"])

        def rsqrt_pool(out, in_, kin, kout, add_eps=True, n=1):
            if add_eps:
                S.op("pool", lambda e, in_=in_: e.tensor_scalar(out=out, in0=in_, scalar1=EPS, scalar2=None, op0=ALU.add),
                     reads=[kin], writes=[kout])
                in_, kin = out, kout
            S.op("pool", lambda e, in_=in_: e.tensor_tensor(out=out, in0=in_, in1=neghalf[:, 0:n], op=ALU.pow),
                 reads=[kin, "neghalf"], writes=[kout])
        S.op("pool", lambda e: e.memset(lg_all[:], 0.0), writes=["lg_all"])
        zt = sb(es, "zt", [128, D], BF16)
        S.op("pool", lambda e: e.memset(zt[:], 0.0), writes=["zt"])

        def zero_fill(ex_):
            if stop_after_phase1:
                return
            S.dma("sp", lambda e: e.dma_start(
                out=xs_d[ex_ * CAP:(ex_ + 1) * CAP, :].rearrange("(b p) d -> p b d", p=128),
                in_=zt[:].unsqueeze(1).to_broadcast([128, NBLK, D])), reads=["zt"], writes=["xz%d" % ex_])

        with ExitStack() as e1:
            w_in = sb(e1, "w_in", [128, 8, C_GA], BF16)
            NG = 4
            wgr = [sb(e1, "wgr%d" % i, [128, 8, 128], BF16) for i in range(NG)]
            wkz = sb(e1, "wkz", [128, 8, 4, 128], BF16)
            wao = sb(e1, "wao", [128, 4, D], BF16)
            wmo = sb(e1, "wmo", [128, 4, D], BF16)
            wout = sb(e1, "wout", [128, 8, D], BF16)
            wr = sb(e1, "wr", [128, 8, 36], F32)
            nmw = sb(e1, "nmw", [128, D], F32)
            nfw = sb(e1, "nfw", [128, D], F32)
            mnw = sb(e1, "mnw", [128, 512], F32)
            cw = sb(e1, "cw", [128, 4, 4], F32)
            cb = sb(e1, "cb", [128, 4], F32)
            bg = sb(e1, "bg", [128, 8], F32)
            esink = sb(e1, "esink", [128, 8], F32)
            maskb = sb(e1, "maskb", [128, 3, 128], BF16)
            cmask8 = sb(e1, "cmask8", [128, 128], F32)
            trif = sb(e1, "trif", [128, 128], F32)
            onesf = sb(e1, "onesf", [128, 128], F32)

            xt = [sb(e1, "xt%d" % i, [128, D], F32) for i in range(2 * BPS)]
            junk = sb(e1, "junk", [128, D], BF16)
            ss = sb(e1, "ss", [128, 1], F32)
            rstd = sb(e1, "rstd", [128, 1], F32)
            hb = sb(e1, "hb", [128, D], BF16)
            hT2 = [sb(e1, "hT%d" % i, [128, 8, SW], BF16) for i in range(2)]
            qT2 = [sb(e1, "qT%d" % i, [128, 4, SW], BF16) for i in range(2)]
            kTz2 = [sb(e1, "kTz%d" % i, [128, 4, 128 + SW], BF16) for i in range(2)]
            Vr2 = [sb(e1, "Vr%d" % i, [128, BPS + 1, 2, 65], BF16) for i in range(2)]
            pre2 = [sb(e1, "pre%d" % i, [128, 4, 3 + SW], F32) for i in range(2)]
            cacc = sb(e1, "cacc", [128, SW], F32)
            qmT = sb(e1, "qmT", [128, 2, SW], BF16)
            kmTz = sb(e1, "kmTz", [128, 2, 2, SW], BF16)
            kmT = sb(e1, "kmT", [128, 2, SW], BF16)
            ktok = sb(e1, "ktok", [128, 2, 128], BF16)
            vm2 = [[sb(e1, "vm%d_%d" % (i, p_), [128, 512], BF16) for i in range(BPS)] for p_ in range(2)]
            so2 = [[sb(e1, "so%d_%d" % (i, p_), [128, 512], F32) for i in range(BPS)] for p_ in range(2)]
            gtt2 = [sb(e1, "gtt%d" % p_, [128, BPS, 8], F32) for p_ in range(2)]
            nlfa = sb(e1, "nlfa", [128, BPS, 4], F32)
            lsu = sb(e1, "lsu", [128, BPS, 4], F32)
            lsz = sb(e1, "lsz", [128, BPS, 4], F32)
            lsz2 = sb(e1, "lsz2", [128, BPS, 4], F32)
            lsq = sb(e1, "lsq", [128, BPS, 4], F32)
            gtmp = sb(e1, "gtmp", [128, 16], F32)
            ex = [sb(e1, "ex%d" % i, [128, 16], F32) for i in range(2)]
            vp = sb(e1, "vp", [128, 4, 129], BF16)
            ATb = sb(e1, "ATb", [128, 4, 128], BF16)
            Yst = sb(e1, "Yst", [128, 4, 129], F32)
            Czb = sb(e1, "Czb", [128, 4, 129], BF16)
            PT = [sb(e1, "PT%d" % i, [128, 4, 128], BF16) for i in range(2)]
            den = sb(e1, "den", [128, 8], F32)
            ya = sb(e1, "ya", [128, 512], BF16)
            ym = sb(e1, "ym", [128, 512], BF16)
            yaT2 = [sb(e1, "yaT%d" % i, [128, 4, SW], BF16) for i in range(2)]
            ymT2 = [sb(e1, "ymT%d" % i, [128, 4, SW], BF16) for i in range(2)]
            absd = sb(e1, "absd", [128, 4], F32)
            ssn = sb(e1, "ssn", [128, 4], F32)
            dm = sb(e1, "dm", [128, 4], F32)
            d2 = sb(e1, "d2", [128, 4], F32)
            sc4 = sb(e1, "sc4", [128, 4], F32)
            sgm = [sb(e1, "sgm%d" % i, [128, SW], F32) for i in range(2)]
            tm = [sb(e1, "tm%d" % i, [128, SW], F32) for i in range(2)]
            mixT = sb(e1, "mixT", [128, 8, SW], BF16)
            h2f = sb(e1, "h2f", [128, D], F32)
            h2b = sb(e1, "h2b", [128, D], BF16)
            h2Th = sb(e1, "h2Th", [128, 8, 128], BF16)
            h2Tl = sb(e1, "h2Tl", [128, 8, 128], BF16)
            wrh = sb(e1, "wrh", [128, 8, 36], BF16)
            wrl = sb(e1, "wrl", [128, 8, 36], BF16)
            ss2 = sb(e1, "ss2", [128, 1], F32)
            rstd2 = sb(e1, "rstd2", [128, 1], F32)

            w_in_v = w_in_d.rearrange("(kc p) n -> p kc n", p=128)
            for c0 in range(0, C_GA, 1024):
                c1 = min(C_GA, c0 + 1024)
                S.dma("pool", lambda e, c0=c0, c1=c1: e.dma_start(out=w_in[:, :, c0:c1], in_=w_in_v[:, :, c0:c1]),
                      writes=["w_in%d" % (c0 // 1024)])
            S.op("pool", lambda e: e.memset(wkz[:], 0.0), writes=["wkz"])
            for kv in range(2):
                for hh in range(2):
                    S.op("dve", lambda e, kv=kv, hh=hh: e.tensor_copy(
                        out=wkz[:, :, kv * 2 + hh, hh * 64:hh * 64 + 64],
                        in_=w_in[:, :, C_AK + kv * 64:C_AK + kv * 64 + 64]), reads=["w_in0", "wkz"], writes=["wkz"])
            S.dma("pool", lambda e: e.dma_start(out=wao[:], in_=wao_d.rearrange("(kc p) n -> p kc n", p=128)),
                  writes=["wao"])
            S.dma("pool", lambda e: e.dma_start(out=wmo[:], in_=wmo_d.rearrange("(kc p) n -> p kc n", p=128)),
                  writes=["wmo"])
            S.dma("pool", lambda e: e.dma_start(out=wout[:], in_=wout_d.rearrange("(kc p) n -> p kc n", p=128)),
                  writes=["wout"])
            ld(wr[:], wr_d.rearrange("(kc p) n -> p kc n", p=128), "wr")
            S.op("dve", lambda e: e.tensor_copy(out=wrh[:], in_=wr[:]), reads=["wr"], writes=["wrh"])
            S.op("dve", lambda e: e.tensor_tensor(out=wrl[:], in0=wr[:], in1=wrh[:], op=ALU.subtract),
                 reads=["wr", "wrh"], writes=["wrl"])
            ld(nmw[:], nmw_d.partition_broadcast(128), "nmw")
            ld(nfw[:], nfw_d.partition_broadcast(128), "nfw")
            ld(mnw[:], mnw_d.partition_broadcast(128), "mnw")
            ld(cw[:], cw_d, "cw")
            ld(cb[:], cb_d, "cb")
            ld(bg[:], bg_d.partition_broadcast(128), "bg")
            ld(esink[:], sk_d.partition_broadcast(128), "esink")
            ld(maskb[:], maskb_d, "maskb")
            ld(cmask8[:], cmask8_d, "cmask8")
            ld(trif[:], trif_d, "trif")
            ld(onesf[:], onesf_d, "onesf")
            S.op("act", lambda e: e.activation(out=esink[:], in_=esink[:], func=AF.Exp, bias=LN2, scale=1.0),
                 reads=["esink"], writes=["esink"])
            S.op("dve", lambda e: e.tensor_scalar(out=mnw[:], in0=mnw[:], scalar1=0.25, scalar2=None, op0=ALU.mult),
                 reads=["mnw"], writes=["mnw"])
            for p_ in range(2):
                S.op("pool", lambda e, p_=p_: e.memset(kTz2[p_][:], 0.0), writes=["kTz%d" % p_])
                S.op("pool", lambda e, p_=p_: e.memset(Vr2[p_][:], 0.0), writes=["Vr%d" % p_])
                S.op("pool", lambda e, p_=p_: e.memset(Vr2[p_][:, :, :, 64:65], 2.0), reads=["Vr%d" % p_], writes=["Vr%d" % p_])
                S.op("pool", lambda e, p_=p_: e.memset(pre2[p_][:], 0.0), writes=["pre%d" % p_])
            S.op("pool", lambda e: e.memset(kmTz[:], 0.0), writes=["kmTz"])
            S.op("pool", lambda e: e.memset(Yst[:], 0.0), writes=["Yst"])
            S.op("pool", lambda e: e.memset(Czb[:], 0.0), writes=["Czb"])
            S.op("pool", lambda e: e.memset(ex[1][:], 1.0), writes=["ex1"])

            def wk(c0, n):
                return ["w_in%d" % c for c in range(c0 // 1024, (c0 + n - 1) // 1024 + 1)]

            def mm(out, lhsT, rhs, st, sp_, R, W):
                S.op("pe", lambda e: e.matmul(out, lhsT=lhsT, rhs=rhs, start=st, stop=sp_), reads=R, writes=W)

            def tr(out, in_, idt, R, W):
                S.op("pe", lambda e: e.transpose(out=out, in_=in_, identity=idt), reads=R, writes=W)

            def xi(s, i):
                return (s % 2) * BPS + i

            def load_x(s, i):
                t0 = s * SW + i * 128
                S.dma("sp", lambda e: e.dma_start(out=xt[xi(s, i)][:], in_=x_d[t0:t0 + 128, :]),
                      writes=["xt%d" % xi(s, i)])

            for s_ in range(min(2, NS)):
                for i in range(BPS):
                    load_x(s_, i)

            evac_flip = [0]

            def evac(out, in_, R, W, scale=None):
                evac_flip[0] ^= 1
                if evac_flip[0]:
                    S.op("act", lambda e: e.copy(out=out, in_=in_), reads=R, writes=W)
                else:
                    S.op("dve", lambda e: e.tensor_copy(out=out, in_=in_), reads=R, writes=W)

            gab_next = [0]
            N_GAB = NS * 16

            def gab_prefetch():
                t = gab_next[0]
                if t >= N_GAB:
                    return
                gab_next[0] += 1
                mm_ = (t % 16) // 2 + 8 * (t % 2)
                S.dma("pool", lambda e: e.dma_start(out=wgr[t % NG][:], in_=wgab_d[mm_]), writes=["wgr%d" % (t % NG)])

            def gab_tile(s_, mt):
                t = 16 * s_ + 2 * (mt % 8) + (1 if mt >= 8 else 0)
                assert t < gab_next[0], "ga/gb tile used before it was prefetched"
                assert t >= gab_next[0] - NG
                return wgr[t % NG], "wgr%d" % (t % NG)

            for _ in range(NG):
                gab_prefetch()

            def gA(s):
                p_ = s % 2
                hT, qT, kTz, Vr, pre, vm, so, gtt = hT2[p_], qT2[p_], kTz2[p_], Vr2[p_], pre2[p_], vm2[p_], so2[p_], gtt2[p_]
                gt = [gtt[:, i, :] for i in range(BPS)]
                kHT, kQT, kKT, kVR, kPRE = "hT%d" % p_, "qT%d" % p_, "kTz%d" % p_, "Vr%d" % p_, "pre%d" % p_
                kVM, kSO, kGT = "vm%d_" + str(p_), "so%d_" + str(p_), "gt%d_" + str(p_)
                for i in range(BPS):
                    xk = "xt%d" % xi(s, i)
                    S.op("act", lambda e, i=i: e.activation(out=hb[:], in_=xt[xi(s, i)][:], func=AF.Square,
                                                            scale=1.0 / 32, accum_out=ss[:]),
                         reads=[xk], writes=["hb", "ss"])
                    rsqrt_pool(rstd[:], ss[:], "ss", "rstd")
                    S.op("dve", lambda e, i=i: e.scalar_tensor_tensor(out=hb[:], in0=xt[xi(s, i)][:], scalar=rstd[:], in1=nmw[:],
                                                                      op0=ALU.mult, op1=ALU.mult),
                         reads=[xk, "rstd", "nmw"], writes=["hb"])
                    yield
                    for kc in range(8):
                        tr(T0[:, kc, :], hb[:, kc * 128:(kc + 1) * 128], identb[:], ["hb", "identb"], ["T0"])
                    evac(hT[:, :, i * 128:(i + 1) * 128], T0[:], ["T0"], [kHT])
                    yield

                if s > 0:
                    q_ = 1 - p_
                    S.op("pool", lambda e: e.tensor_copy(out=kTz[:, :, 0:128], in_=kTz2[q_][:, :, SW:SW + 128]),
                         reads=["kTz%d" % q_], writes=[kKT])
                    S.op("pool", lambda e: e.tensor_copy(out=Vr[:, 0, :, :], in_=Vr2[q_][:, BPS, :, :]),
                         reads=["Vr%d" % q_], writes=[kVR])
                    S.op("pool", lambda e: e.tensor_copy(out=pre[:, :, 0:3], in_=pre2[q_][:, :, SW:SW + 3]),
                         reads=["pre%d" % q_], writes=[kPRE])
                for m in range(12):
                    bt_, ak = nextbank()
                    acc = bt_[:, 0:SW]
                    for kc in range(8):
                        if m < 4:
                            lhsT = w_in[:, kc, C_AQ + m * 128:C_AQ + (m + 1) * 128]
                            wkk = wk(C_AQ + m * 128, 128)
                        elif m < 8:
                            lhsT = wkz[:, kc, m - 4, :]
                            wkk = ["wkz"]
                        else:
                            c0 = C_MQ + (m - 8) * 128
                            lhsT = w_in[:, kc, c0:c0 + 128]
                            wkk = wk(c0, 128)
                        mm(acc, lhsT, hT[:, kc, :], kc == 0, kc == 7, wkk + [kHT], [ak])
                    if m < 4:
                        evac(qT[:, m, :], acc, [ak], [kQT])
                    elif m < 8:
                        evac(kTz[:, m - 4, 128:128 + SW], acc, [ak], [kKT])
                    else:
                        evac(pre[:, m - 8, 3:3 + SW], acc, [ak], [kPRE])
                    yield

                for b in range(BPS):
                    tsl = slice(b * 128, (b + 1) * 128)
                    bv, bvk = nextbank()
                    for kc in range(8):
                        mm(bv[:, 0:128], hT[:, kc, tsl], w_in[:, kc, C_AV:C_AV + 128], kc == 0, kc == 7,
                           [kHT] + wk(C_AV, 128), [bvk])
                    for kc in range(8):
                        mm(bv[:, 128:136], hT[:, kc, tsl], w_in[:, kc, C_MI:C_MI + 8], kc == 0, kc == 7,
                           [kHT] + wk(C_MI, 8), [bvk])
                    S.op("act", lambda e, b=b, bv=bv: e.copy(out=Vr[:, b + 1, :, 0:64],
                                                             in_=bv[:, 0:128].rearrange("p (k d) -> p k d", k=2)),
                         reads=[bvk], writes=[kVR])
                    S.op("dve", lambda e, b=b, bv=bv: e.tensor_tensor(out=gt[b], in0=bv[:, 128:136], in1=bg[:], op=ALU.add),
                         reads=[bvk, "bg"], writes=[kGT % b])
                    yield
                    bm, bmk = nextbank()
                    for kc in range(8):
                        mm(bm[:, :], hT[:, kc, tsl], w_in[:, kc, C_MV:C_MV + 512], kc == 0, kc == 7,
                           [kHT] + wk(C_MV, 512), [bmk])
                    S.op("dve", lambda e, b=b, bm=bm: e.tensor_copy(out=vm[b][:], in_=bm[:]), reads=[bmk], writes=[kVM % b])
                    yield
                    bo, bok = nextbank()
                    for kc in range(8):
                        mm(bo[:, :], hT[:, kc, tsl], w_in[:, kc, C_MO:C_MO + 512], kc == 0, kc == 7,
                           [kHT] + wk(C_MO, 512), [bok])
                    S.op("act", lambda e, b=b, bo=bo: e.activation(out=so[b][:], in_=bo[:], func=AF.Tanh, scale=0.5),
                         reads=[bok], writes=[kSO % b])
                    S.op("dve", lambda e, b=b: e.scalar_tensor_tensor(out=so[b][:], in0=so[b][:], scalar=1.0, in1=mnw[:],
                                                                      op0=ALU.add, op1=ALU.mult),
                         reads=[kSO % b, "mnw"], writes=[kSO % b])

                    yield

            def gG(s):
                p_ = s % 2
                hT, qT, kTz, Vr, pre, vm, so, gtt = hT2[p_], qT2[p_], kTz2[p_], Vr2[p_], pre2[p_], vm2[p_], so2[p_], gtt2[p_]
                gt = [gtt[:, i, :] for i in range(BPS)]
                kHT, kQT, kKT, kVR, kPRE = "hT%d" % p_, "qT%d" % p_, "kTz%d" % p_, "Vr%d" % p_, "pre%d" % p_
                kVM, kSO, kGT = "vm%d_" + str(p_), "so%d_" + str(p_), "gt%d_" + str(p_)
                gks = [kGT % b for b in range(BPS)]
                xf = gtt[:, :, 4:8]
                S.op("act", lambda e: e.activation(out=lsu[:], in_=xf, func=AF.Abs), reads=gks, writes=["lsu"])
                S.op("act", lambda e: e.activation(out=lsu[:], in_=lsu[:], func=AF.Exp, scale=-1.0),
                     reads=["lsu"], writes=["lsu"])
                S.op("dve", lambda e: e.tensor_scalar(out=lsz[:], in0=lsu[:], scalar1=2.0, scalar2=None, op0=ALU.add),
                     reads=["lsu"], writes=["lsz"])
                S.op("dve", lambda e: e.reciprocal(out=lsz[:], in_=lsz[:]), reads=["lsz"], writes=["lsz"])
                S.op("dve", lambda e: e.tensor_tensor(out=lsz[:], in0=lsz[:], in1=lsu[:], op=ALU.mult),
                     reads=["lsz", "lsu"], writes=["lsz"])
                S.op("dve", lambda e: e.tensor_tensor(out=lsz2[:], in0=lsz[:], in1=lsz[:], op=ALU.mult),
                     reads=["lsz"], writes=["lsz2"])
                S.op("dve", lambda e: e.tensor_scalar(out=lsq[:], in0=lsz2[:], scalar1=1.0 / 11, scalar2=None,
                                                      op0=ALU.mult), reads=["lsz2"], writes=["lsq"])
                for cst in (1.0 / 9, 1.0 / 7, 1.0 / 5, 1.0 / 3):
                    S.op("dve", lambda e, cst=cst: e.scalar_tensor_tensor(out=lsq[:], in0=lsq[:], scalar=cst, in1=lsz2[:],
                                                                          op0=ALU.add, op1=ALU.mult),
                         reads=["lsq", "lsz2"], writes=["lsq"])
                S.op("dve", lambda e: e.scalar_tensor_tensor(out=lsq[:], in0=lsq[:], scalar=1.0, in1=lsz[:],
                                                             op0=ALU.add, op1=ALU.mult),
                     reads=["lsq", "lsz"], writes=["lsq"])
                S.op("dve", lambda e: e.tensor_scalar(out=lsu[:], in0=xf, scalar1=0.0, scalar2=None, op0=ALU.min),
                     reads=gks + ["lsu"], writes=["lsu"])
                S.op("dve", lambda e: e.scalar_tensor_tensor(out=nlfa[:], in0=lsq[:], scalar=2.0, in1=lsu[:],
                                                             op0=ALU.mult, op1=ALU.subtract),
                     reads=["lsq", "lsu"], writes=["nlfa"])
                yield

                for t4 in range(4):
                    S.op("dve", lambda e, t4=t4: e.tensor_scalar(out=cacc[:], in0=pre[:, t4, 0:SW],
                                                                 scalar1=cw[:, t4, 0:1], scalar2=cb[:, t4:t4 + 1],
                                                                 op0=ALU.mult, op1=ALU.add),
                         reads=[kPRE, "cw", "cb"], writes=["cacc"])
                    for j in range(1, 4):
                        S.op("dve", lambda e, t4=t4, j=j: e.scalar_tensor_tensor(
                            out=cacc[:], in0=pre[:, t4, j:j + SW], scalar=cw[:, t4, j:j + 1],
                            in1=cacc[:], op0=ALU.mult, op1=ALU.add),
                            reads=[kPRE, "cw", "cacc"], writes=["cacc"])
                    S.op("act", lambda e: e.activation(out=tm[0][:], in_=cacc[:], func=AF.Tanh, scale=0.5),
                         reads=["cacc"], writes=["tm0"])
                    if t4 < 2:
                        S.op("dve", lambda e, t4=t4: e.scalar_tensor_tensor(out=qmT[:, t4, :], in0=tm[0][:], scalar=1.0,
                                                                            in1=cacc[:], op0=ALU.add, op1=ALU.mult),
                             reads=["tm0", "cacc"], writes=["qmT"])
                    else:
                        S.op("dve", lambda e, t4=t4: e.scalar_tensor_tensor(out=kmT[:, t4 - 2, :], in0=tm[0][:], scalar=1.0,
                                                                            in1=cacc[:], op0=ALU.add, op1=ALU.mult),
                             reads=["tm0", "cacc"], writes=["kmT"])
                        for hh in range(2):
                            ps_ = slice(hh * 64, hh * 64 + 64)
                            S.op("pool", lambda e, t4=t4, hh=hh, ps_=ps_: e.tensor_copy(
                                out=kmTz[ps_, t4 - 2, hh, :], in_=kmT[ps_, t4 - 2, :]),
                                reads=["kmT"], writes=["kmTz"])
                    yield

            def gX(s):
                p_ = s % 2
                yaT, ymT, kYA, kYM = yaT2[p_], ymT2[p_], "yaT%d" % p_, "ymT%d" % p_
                hT, qT, kTz, Vr, pre, vm, so, gtt = hT2[p_], qT2[p_], kTz2[p_], Vr2[p_], pre2[p_], vm2[p_], so2[p_], gtt2[p_]
                gt = [gtt[:, i, :] for i in range(BPS)]
                kHT, kQT, kKT, kVR, kPRE = "hT%d" % p_, "qT%d" % p_, "kTz%d" % p_, "Vr%d" % p_, "pre%d" % p_
                kVM, kSO, kGT = "vm%d_" + str(p_), "so%d_" + str(p_), "gt%d_" + str(p_)
                sc3 = SC[:].rearrange("p (a q) -> p a q", a=4)
                pv3 = PV0[:, 0:260].rearrange("p (h d) -> p h d", h=4)
                for b in range(BPS):
                    gb = s * BPS + b
                    qsl = slice(b * 128, (b + 1) * 128)
                    for g in range(2):
                        for j in (2 * g, 2 * g + 1):
                            kv = j // 2
                            ptk = "PT%d" % (j % 2)
                            ptb = PT[j % 2]
                            for hh in range(2):
                                for kt in range(2):
                                    o = sc3[:, hh * 2 + kt, :]
                                    mm(o, kTz[:, kv * 2 + hh, (b + kt) * 128:(b + kt + 1) * 128], qT[:, j, qsl],
                                       True, False, [kKT, kQT], ["SC"])
                                    mi_ = 1 if kt == 1 else (2 if gb == 0 else 0)
                                    mm(o, identb[:], maskb[:, mi_, :], False, True, ["identb", "maskb"], ["SC"])
                            S.op("act", lambda e, ptb=ptb: e.activation(out=ptb[:], in_=sc3, func=AF.Exp, scale=0.125),
                                 reads=["SC"], writes=[ptk])
                            yield
                            for hh in range(2):
                                head = 2 * j + hh
                                o = PV0[:, (head % 4) * 65:(head % 4) * 65 + 65]
                                for kt in range(2):
                                    mm(o, ptb[:, hh * 2 + kt, :], Vr[:, b + kt, kv, :], kt == 0, kt == 1,
                                       [ptk, kVR], ["PV0"])
                            yield
                        S.op("dve", lambda e, g=g: e.tensor_tensor(
                            out=den[:, 4 * g:4 * g + 4], in0=pv3[:, :, 64], in1=esink[:, 4 * g:4 * g + 4], op=ALU.add),
                            reads=["PV0", "esink"], writes=["den"])
                        S.op("dve", lambda e, g=g: e.reciprocal(out=den[:, 4 * g:4 * g + 4], in_=den[:, 4 * g:4 * g + 4]),
                             reads=["den"], writes=["den"])
                        S.op("dve", lambda e, g=g: e.tensor_tensor(
                            out=ya[:, 256 * g:256 * g + 256].rearrange("p (h d) -> p h d", h=4),
                            in0=pv3[:, :, 0:64],
                            in1=den[:, 4 * g:4 * g + 4].unsqueeze(2).to_broadcast([128, 4, 64]), op=ALU.mult),
                            reads=["PV0", "den"], writes=["ya"])
                    yield
                    for c in range(4):
                        tr(T0[:, c, :], ya[:, c * 128:(c + 1) * 128], identb[:], ["ya", "identb"], ["T0"])
                    evac(yaT[:, :, qsl], T0[:, 0:4, :], ["T0"], [kYA])
                    yield
                    if debug:
                        t0 = gb * 128
                        S.dma("sp", lambda e, t0=t0: e.dma_start(out=ya_dbg[t0:t0 + 128, :], in_=ya[:]), reads=["ya"])

            A03 = A0[:].rearrange("p (a q) -> p a q", a=4)
            sqf = junk[:].bitcast(F32).rearrange("p (h d) -> p h d", h=4)
            ND = [A1, U1]
            NDK = ["A1", "U1"]

            def gY(s):
                p_ = s % 2
                yaT, ymT, kYA, kYM = yaT2[p_], ymT2[p_], "yaT%d" % p_, "ymT%d" % p_
                hT, qT, kTz, Vr, pre, vm, so, gtt = hT2[p_], qT2[p_], kTz2[p_], Vr2[p_], pre2[p_], vm2[p_], so2[p_], gtt2[p_]
                gt = [gtt[:, i, :] for i in range(BPS)]
                kHT, kQT, kKT, kVR, kPRE = "hT%d" % p_, "qT%d" % p_, "kTz%d" % p_, "Vr%d" % p_, "pre%d" % p_
                kVM, kSO, kGT = "vm%d_" + str(p_), "so%d_" + str(p_), "gt%d_" + str(p_)
                for b in range(BPS):
                    gb = s * BPS + b
                    csl = slice(b * 128, (b + 1) * 128)
                    gk = kGT % b
                    exc = ex[gb % 2]
                    exk = "ex%d" % (gb % 2)
                    exp_ = ex[(gb + 1) % 2]
                    exkp = "ex%d" % ((gb + 1) % 2)
                    for t2 in range(2):
                        tr(T0[:, t2, :], kmT[:, t2, csl], identb[:], ["kmT", "identb"], ["T0"])
                    S.op("dve", lambda e: e.tensor_copy(out=ktok[:], in_=T0[:, 0:2, :]), reads=["T0"], writes=["ktok"])
                    mm(A1[:, 0:4], trif[:], nlfa[:, b, :], True, True, ["trif", "nlfa"], ["A1"])
                    mm(A1[:, 4:8], onesf[:], nlfa[:, b, :], True, True, ["onesf", "nlfa"], ["A1"])
                    S.op("dve", lambda e, b=b: e.tensor_tensor(out=gtmp[:, 0:4], in0=A1[:, 0:4], in1=gt[b][:, 0:4],
                                                               op=ALU.add), reads=["A1", gk], writes=["gtmp"])
                    S.op("act", lambda e, exc=exc: e.activation(out=exc[:, 4:8], in_=A1[:, 0:4], func=AF.Exp),
                         reads=["A1"], writes=[exk])
                    S.op("act", lambda e, exc=exc: e.activation(out=exc[:, 8:12], in_=A1[:, 4:8], func=AF.Exp,
                                                                bias=lnb[:], scale=-1.0),
                         reads=["A1", "lnb"], writes=[exk])
                    S.op("act", lambda e, exc=exc: e.activation(out=exc[:, 12:16], in_=A1[:, 4:8], func=AF.Exp, scale=-1.0),
                         reads=["A1"], writes=[exk])
                    S.op("act", lambda e, exc=exc: e.activation(out=exc[:, 0:4], in_=gtmp[:, 0:4], func=AF.Exp),
                         reads=["gtmp"], writes=[exk])
                    yield
                    for h in range(4):
                        mm(A03[:, h, :], kmTz[:, h // 2, h % 2, csl], qmT[:, h // 2, csl], True, True,
                           ["kmTz", "qmT"], ["A0"])
                    S.op("dve", lambda e: e.tensor_tensor(out=ATb[:], in0=A03,
                                                          in1=cmask8[:].unsqueeze(1).to_broadcast([128, 4, 128]),
                                                          op=ALU.mult), reads=["A0", "cmask8"], writes=["ATb"])
                    S.op("dve", lambda e, b=b, exc=exc: e.tensor_tensor(
                        out=vp[:, :, 0:128], in0=vm[b][:].rearrange("p (h d) -> p h d", h=4),
                        in1=exc[:, 0:4].unsqueeze(2).to_broadcast([128, 4, 128]), op=ALU.mult),
                        reads=[kVM % b, exk], writes=["vp"])
                    S.op("dve", lambda e, exc=exc: e.tensor_copy(out=vp[:, :, 128], in_=exc[:, 0:4]),
                         reads=[exk], writes=["vp"])
                    yield
                    for h in range(4):
                        g = h // 2
                        o = ND[g][:, (h % 2) * 129:(h % 2) * 129 + 129]
                        mm(o, qmT[:, h // 2, csl], Czb[:, h, :], True, False, ["qmT", "Czb"], [NDK[g]])
                        mm(o, ATb[:, h, :], vp[:, h, :], False, True, ["ATb", "vp"], [NDK[g]])
                    for g in range(2):
                        pv3 = ND[g][:, 0:258].rearrange("p (h d) -> p h d", h=2)
                        S.op("act", lambda e, g=g, pv3=pv3: e.activation(out=absd[:, 2 * g:2 * g + 2], in_=pv3[:, :, 128],
                                                                         func=AF.Abs),
                             reads=[NDK[g]], writes=["absd"])
                        S.op("act", lambda e, g=g, pv3=pv3: e.activation(
                            out=sqf[:, 2 * g:2 * g + 2, :], in_=pv3[:, :, 0:128], func=AF.Square, scale=128.0 ** -0.5),
                            reads=[NDK[g]], writes=["junk"])
                    yield
                    for h in range(4):
                        if h % 2 == 0:
                            for h2_ in (h, h + 1):
                                o = A0[:, (h2_ % 2) * 129:(h2_ % 2) * 129 + 129]
                                mm(o, ktok[:, h2_ // 2, :], vp[:, h2_, :], True, True, ["ktok", "vp"], ["A0"])
                        rs = slice((h % 2) * 64, (h % 2) * 64 + 64)
                        u = A0[rs, (h % 2) * 129:(h % 2) * 129 + 129]
                        S.op("dve", lambda e, h=h, rs=rs, u=u, exp_=exp_: e.scalar_tensor_tensor(
                            out=Yst[rs, h, :], in0=Yst[rs, h, :], scalar=exp_[rs, 12 + h:13 + h], in1=u,
                            op0=ALU.mult, op1=ALU.add), reads=["Yst", exkp, "A0"], writes=["Yst"])
                        S.op("act", lambda e, h=h, rs=rs, exc=exc: e.activation(
                            out=Czb[rs, h, :], in_=Yst[rs, h, :], func=AF.Copy, scale=exc[rs, 8 + h:9 + h]),
                            reads=["Yst", exk], writes=["Czb"])
                        if h == 1:
                            yield
                    S.op("dve", lambda e: e.tensor_reduce(out=ssn[:], in_=sqf, axis=AX.X, op=ALU.add),
                         reads=["junk"], writes=["ssn"])
                    S.op("dve", lambda e, exc=exc: e.tensor_tensor(out=dm[:], in0=absd[:], in1=exc[:, 4:8], op=ALU.max),
                         reads=["absd", exk], writes=["dm"])
                    S.op("dve", lambda e: e.tensor_tensor(out=d2[:], in0=dm[:], in1=dm[:], op=ALU.mult),
                         reads=["dm"], writes=["d2"])
                    S.op("dve", lambda e: e.scalar_tensor_tensor(out=d2[:], in0=d2[:], scalar=EPS, in1=ssn[:],
                                                                  op0=ALU.mult, op1=ALU.add),
                         reads=["d2", "ssn"], writes=["d2"])
                    rsqrt_pool(sc4[:], d2[:], "d2", "sc4", add_eps=False, n=4)
                    yield
                    for h in range(4):
                        g = h // 2
                        o = ND[g][:, (h % 2) * 129:(h % 2) * 129 + 128]
                        S.op("dve", lambda e, h=h, o=o, b=b: e.scalar_tensor_tensor(
                            out=ym[:, h * 128:(h + 1) * 128], in0=o, scalar=sc4[:, h:h + 1],
                            in1=so[b][:, h * 128:(h + 1) * 128], op0=ALU.mult, op1=ALU.mult),
                            reads=[NDK[g], "sc4", kSO % b], writes=["ym"])
                    yield
                    for c in range(4):
                        tr(T0[:, 4 + c, :], ym[:, c * 128:(c + 1) * 128], identb[:], ["ym", "identb"], ["T0"])
                    evac(ymT[:, :, csl], T0[:, 4:8, :], ["T0"], [kYM])
                    yield
                    if debug:
                        t0 = gb * 128
                        S.dma("sp", lambda e, t0=t0: e.dma_start(out=ym_dbg[t0:t0 + 128, :], in_=ym[:]), reads=["ym"])

            def gC(s):
                p_ = s % 2
                yaT, ymT, kYA, kYM = yaT2[p_], ymT2[p_], "yaT%d" % p_, "ymT%d" % p_
                hT, qT, kTz, Vr, pre, vm, so, gtt = hT2[p_], qT2[p_], kTz2[p_], Vr2[p_], pre2[p_], vm2[p_], so2[p_], gtt2[p_]
                gt = [gtt[:, i, :] for i in range(BPS)]
                kHT, kQT, kKT, kVR, kPRE = "hT%d" % p_, "qT%d" % p_, "kTz%d" % p_, "Vr%d" % p_, "pre%d" % p_
                kVM, kSO, kGT = "vm%d_" + str(p_), "so%d_" + str(p_), "gt%d_" + str(p_)
                for m in range(8):
                    msl = slice(m * 128, (m + 1) * 128)
                    bka, ka = nextbank()
                    bkb, kb = nextbank()
                    a0 = bka[:, 0:SW]
                    a1 = bka[:, SW:2 * SW]
                    b0 = bkb[:, 0:SW]
                    b1 = bkb[:, SW:2 * SW]
                    ta = gab_tile(s, m)
                    tb = gab_tile(s, 8 + m)
                    for kc in range(8):
                        mm(a0, ta[0][:, kc, :], hT[:, kc, :], kc == 0, kc == 7, [ta[1], kHT], [ka])
                    for kc in range(4):
                        mm(a1, wao[:, kc, msl], yaT[:, kc, :], kc == 0, kc == 3, ["wao", kYA], [ka])
                    for kc in range(8):
                        mm(b0, tb[0][:, kc, :], hT[:, kc, :], kc == 0, kc == 7, [tb[1], kHT], [kb])
                    for kc in range(4):
                        mm(b1, wmo[:, kc, msl], ymT[:, kc, :], kc == 0, kc == 3, ["wmo", kYM], [kb])
                    gab_prefetch()
                    gab_prefetch()
                    S.op("act", lambda e, a0=a0: e.activation(out=sgm[0][:], in_=a0, func=AF.Tanh, scale=0.5),
                         reads=[ka], writes=["sgm0"])
                    S.op("act", lambda e, b0=b0: e.activation(out=sgm[1][:], in_=b0, func=AF.Tanh, scale=0.5),
                         reads=[kb], writes=["sgm1"])
                    S.op("dve", lambda e, a1=a1: e.scalar_tensor_tensor(out=tm[0][:], in0=sgm[0][:], scalar=1.0, in1=a1,
                                                                        op0=ALU.add, op1=ALU.mult),
                         reads=[ka, "sgm0"], writes=["tm0"])
                    S.op("dve", lambda e, b1=b1: e.scalar_tensor_tensor(out=tm[1][:], in0=sgm[1][:], scalar=1.0, in1=b1,
                                                                        op0=ALU.add, op1=ALU.mult),
                         reads=[kb, "sgm1"], writes=["tm1"])
                    S.op("dve", lambda e, m=m: e.tensor_tensor(out=mixT[:, m, :], in0=tm[0][:], in1=tm[1][:], op=ALU.add),
                         reads=["tm0", "tm1"], writes=["mixT"])
                    yield

            def gD(s):
                for b in range(BPS):
                    gb = s * BPS + b
                    t0 = gb * 128
                    tsl = slice(b * 128, (b + 1) * 128)
                    for half in range(2):
                        bo_, bok_ = nextbank()
                        for kc in range(8):
                            mm(bo_[:], mixT[:, kc, tsl], wout[:, kc, half * 512:(half + 1) * 512], kc == 0, kc == 7,
                               ["mixT", "wout"], [bok_])
                        S.op("dve", lambda e, b=b, half=half, bo_=bo_: e.tensor_tensor(
                            out=xt[xi(s, b)][:, half * 512:(half + 1) * 512], in0=bo_[:],
                            in1=xt[xi(s, b)][:, half * 512:(half + 1) * 512], op=ALU.add),
                            reads=[bok_, "xt%d" % xi(s, b)], writes=["xt%d" % xi(s, b)])
                        yield
                    x2 = xt[xi(s, b)]
                    xk2 = "xt%d" % xi(s, b)
                    S.dma("sp", lambda e, t0=t0, x2=x2: e.dma_start(out=x2_d[t0:t0 + 128, :], in_=x2[:]), reads=[xk2])
                    S.op("act", lambda e, x2=x2: e.activation(out=h2b[:], in_=x2[:], func=AF.Square, scale=1.0 / 32,
                                                              accum_out=ss2[:]), reads=[xk2], writes=["h2b", "ss2"])
                    rsqrt_pool(rstd2[:], ss2[:], "ss2", "rstd2")
                    S.op("dve", lambda e, x2=x2: e.scalar_tensor_tensor(out=h2f[:], in0=x2[:], scalar=rstd2[:], in1=nfw[:],
                                                                        op0=ALU.mult, op1=ALU.mult),
                         reads=[xk2, "rstd2", "nfw"], writes=["h2f"])
                    if s + 2 < NS:
                        load_x(s + 2, b)
                    yield
                    S.op("act", lambda e: e.copy(out=h2b[:], in_=h2f[:]), reads=["h2f"], writes=["h2b"])
                    S.dma("sp", lambda e, t0=t0: e.dma_start(out=h2_d[t0:t0 + 128, :], in_=h2b[:]), reads=["h2b"])
                    S.op("dve", lambda e: e.tensor_tensor(out=hb[:], in0=h2f[:], in1=h2b[:], op=ALU.subtract),
                         reads=["h2f", "h2b"], writes=["hb"])
                    for src, srck, dst, dstk in ((h2b, "h2b", h2Th, "h2Th"), (hb, "hb", h2Tl, "h2Tl")):
                        bt2, bk2 = nextbank()
                        btb = bt2[:].bitcast(BF16).rearrange("p (c t) -> p c t", c=8)
                        for kc in range(8):
                            tr(btb[:, kc, :], src[:, kc * 128:(kc + 1) * 128], identb[:], [srck, "identb"], [bk2])
                        evac(dst[:], btb, [bk2], [dstk])
                        yield
                    bt3, bk3 = nextbank()
                    for kc in range(8):
                        mm(bt3[:, 0:36], h2Th[:, kc, :], wrh[:, kc, :], kc == 0, False, ["h2Th", "wrh"], [bk3])
                        mm(bt3[:, 0:36], h2Tl[:, kc, :], wrh[:, kc, :], False, False, ["h2Tl", "wrh"], [bk3])
                        mm(bt3[:, 0:36], h2Th[:, kc, :], wrl[:, kc, :], False, kc == 7, ["h2Th", "wrl"], [bk3])
                    S.op("dve", lambda e, gb=gb, bt3=bt3: e.tensor_copy(out=lg_all[:, gb, :], in_=bt3[:, 0:36]),
                         reads=[bk3], writes=["lg_all"])
                    yield

            RW = [int(x_) for x_ in os.environ.get("K_RW", "1,1,1").split(",")]

            def run(*gens):
                act_ = [(g_, RW[i_] if len(gens) == 3 else 1) for i_, g_ in enumerate(gens)]
                while act_:
                    for ent in list(act_):
                        for _ in range(ent[1]):
                            try:
                                next(ent[0])
                            except StopIteration:
                                act_.remove(ent)
                                break

            def chain(*gens):
                for g_ in gens:
                    yield from g_

            run(gA(0))
            run(chain(gG(0), gY(0)), gX(0), *([gA(1)] if NS > 1 else []))
            zf = 0
            for s in range(NS):
                for _ in range(2):
                    if zf < NE:
                        zero_fill(zf)
                        zf += 1
                side = [gC(s), gD(s)]
                if s + 2 < NS:
                    side.append(gA(s + 2))
                streams = []
                if s + 1 < NS:
                    streams = [chain(gG(s + 1), gY(s + 1)), gX(s + 1)]
                run(*streams, chain(*side))

        S.barrier()
        if debug:
            S.dma("sp", lambda e: e.dma_start(out=lg_d, in_=lg_all[:]), reads=["lg_all"])

        if not stop_after_phase1:
            with ExitStack() as e2:
                wgt = [sb(e2, "wgt%d" % i, [128, 8, 512], BF16) for i in range(2)]
                wut = [sb(e2, "wut%d" % i, [128, 8, 512], BF16) for i in range(2)]
                wdt = [sb(e2, "wdt%d" % i, [128, 4, D], BF16) for i in range(2)]
                def load_expert(ex_):
                    bi = ex_ % 2
                    S.dma("pool", lambda e: e.dma_start(out=wgt[bi][:],
                                                        in_=wg_d[ex_].rearrange("(kc p) n -> p kc n", p=128)),
                          writes=["wgt%d" % bi])
                    S.dma("pool", lambda e: e.dma_start(out=wut[bi][:],
                                                        in_=wu_d[ex_].rearrange("(kc p) n -> p kc n", p=128)),
                          writes=["wut%d" % bi])
                    S.dma("pool", lambda e: e.dma_start(out=wdt[bi][:],
                                                        in_=wd_d[ex_].rearrange("(kc p) n -> p kc n", p=128)),
                          writes=["wdt%d" % bi])

                load_expert(0)
                load_expert(1)
                brt = sb(e2, "brt", [128, 36], F32)
                eoff = sb(e2, "eoff", [128, NE], F32)
                nlw = sb(e2, "nlw", [128, D], F32)
                ld(brt[:], brt_d.partition_broadcast(128), "brt")
                ld(eoff[:], eoff_d.partition_broadcast(128), "eoff")
                ld(nlw[:], nlw_d.partition_broadcast(128), "nlw")
                gmax = sb(e2, "gmax", [128, NT], F32)
                gsh = sb(e2, "gsh", [128, NT, 4], F32)
                goh = sb(e2, "goh", [128, NT, 4], F32)
                gsum = sb(e2, "gsum", [128, NT], F32)
                gp = sb(e2, "gp", [128, NT], F32)
                elm = sb(e2, "elm", [128, NT, 32], F32)
                pen = sb(e2, "pen", [128, NT, 4], F32)
                m1 = sb(e2, "m1", [128, NT], F32)
                m2 = sb(e2, "m2", [128, NT], F32)
                oh1 = sb(e2, "oh1", [128, NT, 32], F32)
                oh2 = sb(e2, "oh2", [128, NT, 32], F32)
                Mb = sb(e2, "Mb", [128, NT * 32], BF16)
                pos = sb(e2, "pos", [128, NT, 32], F32)
                base = sb(e2, "base", [128, NT, 32], F32)
                tmpe = sb(e2, "tmpe", [128, NT, 32], F32)
                dstf = sb(e2, "dstf", [128, 2, NT], F32)
                dsti = sb(e2, "dsti", [128, 2, NT], I32)
                wts = sb(e2, "wts", [128, 2, NT], F32)
                dlt = sb(e2, "dlt", [128, NT], F32)

                def dv(fn, R, W):
                    S.op("dve", fn, reads=R, writes=W)

                dv(lambda e: e.tensor_tensor(out=lg_all[:], in0=lg_all[:],
                                             in1=brt[:].unsqueeze(1).to_broadcast([128, NT, 36]), op=ALU.add),
                   ["lg_all", "brt"], ["lg_all"])
                gl = lg_all[:, :, 0:4]
                el = lg_all[:, :, 4:36]
                dv(lambda e: e.tensor_reduce(out=gmax[:], in_=gl, axis=AX.X, op=ALU.max), ["lg_all"], ["gmax"])
                dv(lambda e: e.tensor_tensor(out=gsh[:], in0=gl, in1=gmax[:].unsqueeze(2).to_broadcast([128, NT, 4]),
                                             op=ALU.subtract), ["lg_all", "gmax"], ["gsh"])
                dv(lambda e: e.tensor_single_scalar(out=goh[:], in_=gsh[:], scalar=0.0, op=ALU.is_ge),
                   ["gsh"], ["goh"])
                S.op("act", lambda e: e.activation(out=gsh[:], in_=gsh[:], func=AF.Exp), reads=["gsh"], writes=["gsh"])
                dv(lambda e: e.tensor_reduce(out=gsum[:], in_=gsh[:], axis=AX.X, op=ALU.add), ["gsh"], ["gsum"])
                dv(lambda e: e.reciprocal(out=gp[:], in_=gsum[:]), ["gsum"], ["gp"])
                dv(lambda e: e.tensor_scalar(out=pen[:], in0=goh[:], scalar1=1e30, scalar2=-1e30, op0=ALU.mult,
                                             op1=ALU.add), ["goh"], ["pen"])
                dv(lambda e: e.tensor_tensor(out=elm[:].rearrange("p j (g k) -> p j g k", g=4),
                                             in0=el.rearrange("p j (g k) -> p j g k", g=4),
                                             in1=pen[:].unsqueeze(3).to_broadcast([128, NT, 4, 8]), op=ALU.add),
                   ["lg_all", "pen"], ["elm"])
                dv(lambda e: e.tensor_reduce(out=m1[:], in_=elm[:], axis=AX.X, op=ALU.max), ["elm"], ["m1"])
                dv(lambda e: e.tensor_tensor(out=oh1[:], in0=elm[:], in1=m1[:].unsqueeze(2).to_broadcast([128, NT, 32]),
                                             op=ALU.is_ge), ["elm", "m1"], ["oh1"])
                dv(lambda e: e.scalar_tensor_tensor(out=elm[:], in0=oh1[:], scalar=-1e30, in1=elm[:], op0=ALU.mult,
                                                    op1=ALU.add), ["oh1", "elm"], ["elm"])
                dv(lambda e: e.tensor_reduce(out=m2[:], in_=elm[:], axis=AX.X, op=ALU.max), ["elm"], ["m2"])
                dv(lambda e: e.tensor_tensor(out=oh2[:], in0=elm[:], in1=m2[:].unsqueeze(2).to_broadcast([128, NT, 32]),
                                             op=ALU.is_ge), ["elm", "m2"], ["oh2"])
                dv(lambda e: e.tensor_tensor(out=dlt[:], in0=m1[:], in1=m2[:], op=ALU.subtract), ["m1", "m2"], ["dlt"])
                S.op("act", lambda e: e.activation(out=dlt[:], in_=dlt[:], func=AF.Sigmoid), reads=["dlt"], writes=["dlt"])
                dv(lambda e: e.tensor_tensor(out=wts[:, 0, :], in0=dlt[:], in1=gp[:], op=ALU.mult),
                   ["dlt", "gp"], ["wts"])
                dv(lambda e: e.tensor_tensor(out=wts[:, 1, :], in0=gp[:], in1=wts[:, 0, :], op=ALU.subtract),
                   ["gp", "wts"], ["wts"])
                dv(lambda e: e.tensor_tensor(out=Mb[:].rearrange("p (j e) -> p j e", j=NT), in0=oh1[:], in1=oh2[:],
                                             op=ALU.add), ["oh1", "oh2"], ["Mb"])
                for hf in range(2):
                    S.op("pe", lambda e, hf=hf: e.matmul(AA[hf][:], lhsT=trisb[:], rhs=Mb[:, hf * 512:(hf + 1) * 512],
                                                         start=True, stop=True), reads=["trisb", "Mb"], writes=["A%d" % hf])
                    S.op("pe", lambda e, hf=hf: e.matmul(UU[hf][:], lhsT=onesb[:], rhs=Mb[:, hf * 512:(hf + 1) * 512],
                                                         start=True, stop=True), reads=["onesb", "Mb"], writes=["U%d" % hf])
                    dv(lambda e, hf=hf: e.tensor_copy(out=pos[:, hf * 16:(hf + 1) * 16, :],
                                                      in_=AA[hf][:].rearrange("p (j e) -> p j e", j=16)),
                       ["A%d" % hf], ["pos"])
                    dv(lambda e, hf=hf: e.tensor_copy(out=tmpe[:, hf * 16:(hf + 1) * 16, :],
                                                      in_=UU[hf][:].rearrange("p (j e) -> p j e", j=16)),
                       ["U%d" % hf], ["tmpe"])
                dv(lambda e: e.memset(base[:, 0, :], 0.0), [], ["base"])
                for j in range(1, NT):
                    dv(lambda e, j=j: e.tensor_tensor(out=base[:, j, :], in0=base[:, j - 1, :], in1=tmpe[:, j - 1, :],
                                                      op=ALU.add), ["base", "tmpe"], ["base"])
                dv(lambda e: e.tensor_tensor(out=pos[:], in0=pos[:], in1=base[:], op=ALU.add), ["pos", "base"], ["pos"])
                dv(lambda e: e.tensor_scalar(out=pos[:], in0=pos[:], scalar1=float(CAP - 1), scalar2=None, op0=ALU.min),
                   ["pos"], ["pos"])
                dv(lambda e: e.tensor_tensor(out=pos[:], in0=pos[:], in1=eoff[:].unsqueeze(1).to_broadcast([128, NT, NE]),
                                             op=ALU.add), ["pos", "eoff"], ["pos"])
                for k, oh in enumerate((oh1, oh2)):
                    dv(lambda e, oh=oh: e.tensor_tensor(out=tmpe[:], in0=pos[:], in1=oh[:], op=ALU.mult),
                       ["pos", "oh1", "oh2", "tmpe"], ["tmpe"])
                    dv(lambda e, k=k: e.tensor_reduce(out=dstf[:, k, :], in_=tmpe[:], axis=AX.X, op=ALU.add),
                       ["tmpe"], ["dstf"])
                dv(lambda e: e.tensor_copy(out=dsti[:], in_=dstf[:]), ["dstf"], ["dsti"])
                if debug:
                    S.dma("sp", lambda e: e.dma_start(out=dst_d, in_=dsti[:]), reads=["dsti"])
                    S.dma("sp", lambda e: e.dma_start(out=wts_d, in_=wts[:]), reads=["wts"])

                NHR = 8
                hrow = [sb(e2, "hrow%d" % i, [128, D], BF16) for i in range(NHR)]
                for j in range(NT):
                    hr = hrow[j % NHR]
                    hk = "hrow%d" % (j % NHR)
                    S.dma("sp", lambda e, j=j, hr=hr: e.dma_start(out=hr[:], in_=h2_d[j * 128:(j + 1) * 128, :]),
                          writes=[hk])
                    for k in range(2):
                        S.dma("pool", lambda e, j=j, k=k, hr=hr: e.indirect_dma_start(
                            out=xs_d, out_offset=bass.IndirectOffsetOnAxis(ap=dsti[:, k, j:j + 1], axis=0),
                            in_=hr[:], in_offset=None), reads=[hk, "dsti"] + ["xz%d" % z for z in range(NE)])

                S.barrier()
                xsb = [sb(e2, "xsb%d" % i, [128, NBLK, D], BF16) for i in range(3)]
                xsT = [sb(e2, "xsT%d" % i, [128, 8, CAP], BF16) for i in range(2)]
                sg = [sb(e2, "sg%d" % i, [128, CAP], F32) for i in range(2)]
                aT = [sb(e2, "aT%d" % i, [128, 4, CAP], BF16) for i in range(2)]
                ysb = [sb(e2, "ysb%d" % i, [128, D], F32) for i in range(3)]

                def load_xs(ex_):
                    b3 = ex_ % 3
                    S.dma("sp", lambda e: e.dma_start(
                        out=xsb[b3][:], in_=xs_d[ex_ * CAP:(ex_ + 1) * CAP, :].rearrange("(b p) d -> p b d", p=128)),
                        writes=["xsb%d" % b3])

                def transposes(ex_):
                    b3, b2 = ex_ % 3, ex_ % 2
                    for blk in range(NBLK):
                        for kc in range(8):
                            S.op("pe", lambda e, blk=blk, kc=kc: e.transpose(
                                out=T0[:, kc, :], in_=xsb[b3][:, blk, kc * 128:(kc + 1) * 128], identity=identb[:]),
                                reads=["xsb%d" % b3, "identb"], writes=["T0"])
                        if blk % 2 == 0:
                            S.op("act", lambda e, blk=blk: e.copy(out=xsT[b2][:, :, blk * 128:(blk + 1) * 128], in_=T0[:]),
                                 reads=["T0"], writes=["xsT%d" % b2])
                        else:
                            S.op("dve", lambda e, blk=blk: e.tensor_copy(out=xsT[b2][:, :, blk * 128:(blk + 1) * 128],
                                                                         in_=T0[:]), reads=["T0"], writes=["xsT%d" % b2])

                GU_B = [((A0, "A0"), (A1, "A1")), ((SC, "SC"), (PV0, "PV0"))]
                DN_B = [(U0, "U0"), (U1, "U1"), (PV1, "PV1")]
                dn_i = [0]

                load_xs(0)
                load_xs(1)
                transposes(0)
                yflip = 0
                for ex_ in range(NE):
                    bi = ex_ % 2
                    if ex_ + 1 < NE and ex_ + 1 >= 2:
                        load_expert(ex_ + 1)
                    if ex_ + 2 < NE:
                        load_xs(ex_ + 2)
                    xT = xsT[bi]
                    xk = "xsT%d" % bi
                    aTb = aT[bi]
                    ak_ = "aT%d" % bi
                    for dc in range(4):
                        dsl = slice(dc * 128, (dc + 1) * 128)
                        (bg_, kg_), (bu_, ku_) = GU_B[dc % 2]
                        for kc in range(8):
                            S.op("pe", lambda e, kc=kc, dsl=dsl, bg_=bg_, bi=bi, xT=xT: e.matmul(
                                bg_[:, 0:CAP], lhsT=wgt[bi][:, kc, dsl], rhs=xT[:, kc, :], start=(kc == 0), stop=(kc == 7)),
                                reads=["wgt%d" % bi, xk], writes=[kg_])
                        for kc in range(8):
                            S.op("pe", lambda e, kc=kc, dsl=dsl, bu_=bu_, bi=bi, xT=xT: e.matmul(
                                bu_[:, 0:CAP], lhsT=wut[bi][:, kc, dsl], rhs=xT[:, kc, :], start=(kc == 0), stop=(kc == 7)),
                                reads=["wut%d" % bi, xk], writes=[ku_])
                        sgb = sg[dc % 2]
                        S.op("act", lambda e, sgb=sgb, bg_=bg_: e.activation(out=sgb[:], in_=bg_[:, 0:CAP], func=AF.Silu),
                             reads=[kg_], writes=["sg%d" % (dc % 2)])
                        dv(lambda e, dc=dc, sgb=sgb, bu_=bu_, aTb=aTb: e.tensor_tensor(out=aTb[:, dc, :], in0=bu_[:, 0:CAP], in1=sgb[:],
                                                                             op=ALU.mult),
                           [ku_, "sg%d" % (dc % 2)], [ak_])
                    if ex_ + 1 < NE:
                        transposes(ex_ + 1)
                    for blk in range(NBLK):
                        yb = ysb[yflip]
                        yk = "ysb%d" % yflip
                        yflip = (yflip + 1) % 3
                        for half in range(2):
                            bd_, kd_ = DN_B[dn_i[0] % 3]
                            dn_i[0] += 1
                            for dc in range(4):
                                S.op("pe", lambda e, blk=blk, half=half, dc=dc, bd_=bd_, bi=bi, aTb=aTb: e.matmul(
                                    bd_[:], lhsT=aTb[:, dc, blk * 128:(blk + 1) * 128],
                                    rhs=wdt[bi][:, dc, half * 512:(half + 1) * 512], start=(dc == 0), stop=(dc == 3)),
                                    reads=[ak_, "wdt%d" % bi], writes=[kd_])
                            if half == 0:
                                S.op("act", lambda e, yb=yb, bd_=bd_: e.copy(out=yb[:, 0:512], in_=bd_[:]), reads=[kd_], writes=[yk])
                            else:
                                dv(lambda e, yb=yb, bd_=bd_: e.tensor_copy(out=yb[:, 512:1024], in_=bd_[:]), [kd_], [yk])
                        r0 = ex_ * CAP + blk * 128
                        S.dma("sp", lambda e, r0=r0, yb=yb: e.dma_start(out=ys_d[r0:r0 + 128, :], in_=yb[:]),
                              reads=[yk])

                S.barrier()
                NB = 4
                y0 = [sb(e2, "y0_%d" % i, [128, D], F32) for i in range(NB)]
                y1 = [sb(e2, "y1_%d" % i, [128, D], F32) for i in range(NB)]
                xr = [sb(e2, "xr%d" % i, [128, D], F32) for i in range(NB)]
                jk2 = sb(e2, "jk2", [128, D], BF16)
                ss3 = [sb(e2, "ss3_%d" % i, [128, 1], F32) for i in range(2)]
                rs3 = [sb(e2, "rs3_%d" % i, [128, 1], F32) for i in range(2)]
                def comb_loads(j):
                    bi = j % NB
                    S.dma("sp", lambda e: e.dma_start(out=xr[bi][:], in_=x2_d[j * 128:(j + 1) * 128, :]),
                          writes=["xr%d" % bi])
                    for k, yy in enumerate((y0, y1)):
                        S.dma("pool", lambda e, k=k, yy=yy: e.indirect_dma_start(
                            out=yy[bi][:], out_offset=None, in_=ys_d,
                            in_offset=bass.IndirectOffsetOnAxis(ap=dsti[:, k, j:j + 1], axis=0)),
                            reads=["dsti"], writes=["y%d_%d" % (k, bi)])

                for j in range(NB - 1):
                    comb_loads(j)
                for j in range(NT):
                    bi = j % NB
                    b2 = j % 2
                    if j + NB - 1 < NT:
                        comb_loads(j + NB - 1)
                    dv(lambda e, j=j, bi=bi: e.scalar_tensor_tensor(out=xr[bi][:], in0=y0[bi][:], scalar=wts[:, 0, j:j + 1],
                                                                    in1=xr[bi][:], op0=ALU.mult, op1=ALU.add),
                       ["y0_%d" % bi, "wts", "xr%d" % bi], ["xr%d" % bi])
                    dv(lambda e, j=j, bi=bi: e.scalar_tensor_tensor(out=xr[bi][:], in0=y1[bi][:], scalar=wts[:, 1, j:j + 1],
                                                                    in1=xr[bi][:], op0=ALU.mult, op1=ALU.add),
                       ["y1_%d" % bi, "wts", "xr%d" % bi], ["xr%d" % bi])
                    S.op("act", lambda e, bi=bi, b2=b2: e.activation(out=jk2[:], in_=xr[bi][:], func=AF.Square,
                                                                     scale=1.0 / 32, accum_out=ss3[b2][:]),
                         reads=["xr%d" % bi], writes=["jk2", "ss3_%d" % b2])
                    S.op("act", lambda e, b2=b2: e.activation(out=rs3[b2][:], in_=ss3[b2][:], func=AF.Sqrt, bias=epsb[:],
                                                              scale=1.0),
                         reads=["ss3_%d" % b2, "epsb"], writes=["rs3_%d" % b2])
                    dv(lambda e, b2=b2: e.reciprocal(out=rs3[b2][:], in_=rs3[b2][:]), ["rs3_%d" % b2], ["rs3_%d" % b2])
                    dv(lambda e, bi=bi, b2=b2: e.scalar_tensor_tensor(out=y0[bi][:], in0=xr[bi][:], scalar=rs3[b2][:],
                                                                      in1=nlw[:], op0=ALU.mult, op1=ALU.mult),
                       ["xr%d" % bi, "rs3_%d" % b2, "nlw"], ["y0_%d" % bi])
                    S.dma("sp", lambda e, j=j, bi=bi: e.dma_start(out=out_d[j * 128:(j + 1) * 128, :], in_=y0[bi][:]),
                          reads=["y0_%d" % bi])
        S.barrier()
        S.emit()
        print("total ops", S.nops, {k: S.cnt[k] for k in S.ENG})
    return nc


def make_consts():
    bf = ml_dtypes.bfloat16
    i = np.arange(128)
    s_le_t = (i[:, None] <= i[None, :])
    maskb = np.zeros((128, 3, 128), np.float32)
    maskb[:, 0, :] = np.where(i[:, None] > i[None, :], 0.0, -30000.0)
    maskb[:, 1, :] = np.where(i[:, None] <= i[None, :], 0.0, -30000.0)
    maskb[:, 2, :] = -30000.0
    return {
        "ident_bf": np.eye(128, dtype=np.float32).astype(bf),
        "ident_f": np.eye(128, dtype=np.float32),
        "maskb": maskb.astype(bf),
        "cmask8": (0.03125 * s_le_t).astype(np.float32),
        "tri_f": s_le_t.astype(np.float32),
        "ones_f": np.ones((128, 128), np.float32),
        "tris_bf": (i[:, None] < i[None, :]).astype(np.float32).astype(bf),
        "ones_bf": np.ones((128, 128), np.float32).astype(bf),
        "eoff": (np.arange(NE, dtype=np.float32) * CAP)[None, :],
    }


def make_in_maps(inputs, cores):
    f = lambda a: np.ascontiguousarray(np.asarray(a, dtype=np.float32))
    x = f(inputs["x"])
    conv_w = f(inputs["conv_w"])[0]
    conv_b = f(inputs["conv_b"])[0]
    shared = {
        "w_in": f(inputs["w_in"])[0],
        "w_gab": np.ascontiguousarray(
            f(inputs["w_in"])[0][:, C_GA:C_GA + 2048].reshape(8, 128, 16, 128).transpose(2, 1, 0, 3)),
        "w_attn_o": f(inputs["w_attn_o"])[0],
        "w_mlstm_o": f(inputs["w_mlstm_o"])[0],
        "w_out": f(inputs["w_out"])[0],
        "w_rt": np.ascontiguousarray(np.concatenate([f(inputs["w_group"])[0], f(inputs["w_router"])[0]], axis=1)),
        "w_gate": f(inputs["w_gate"])[0],
        "w_up": f(inputs["w_up"])[0],
        "w_down": f(inputs["w_down"])[0],
        "norm_mix_w": f(inputs["norm_mix_w"]).reshape(1, D),
        "norm_ffn_w": f(inputs["norm_ffn_w"]).reshape(1, D),
        "norm_final_w": f(inputs["norm_final_w"]).reshape(1, D),
        "mlstm_norm_w": f(inputs["mlstm_norm_w"]).reshape(1, 512),
        "conv_wt": np.ascontiguousarray(conv_w.reshape(4, 4, 128).transpose(2, 1, 0)),
        "conv_bt": np.ascontiguousarray(conv_b.reshape(4, 128).T),
        "b_gates": np.ascontiguousarray(np.concatenate([f(inputs["b_igate"])[0], f(inputs["b_fgate"])[0]])[None, :]),
        "attn_sinks": f(inputs["attn_sinks"]).reshape(1, 8),
        "b_rt": np.ascontiguousarray(np.concatenate([f(inputs["b_group"])[0], f(inputs["b_router"])[0]])[None, :]),
    }
    shared.update(make_consts())
    return [dict(shared, x=np.ascontiguousarray(x[c])) for c in cores]


def kernel(**inputs):
    nc = build_program()
    in_maps = make_in_maps(inputs, range(8))
    res = run_bass_kernel_spmd(nc, in_maps, core_ids=list(range(8)))
    return np.stack([np.asarray(r["out"], dtype=np.float32) for r in res.results], axis=0)
```

```python
import os
import numpy as np
import ml_dtypes
from contextlib import ExitStack
import concourse.bass as bass
import concourse.mybir as mybir
from concourse.bass_utils import run_bass_kernel_spmd

F32 = mybir.dt.float32
BF16 = mybir.dt.bfloat16
I32 = mybir.dt.int32
ALU = mybir.AluOpType
AF = mybir.ActivationFunctionType
AX = mybir.AxisListType

T = 4096
D = 1024
NT = 32
SW = 256
NS = int(os.environ.get('K_NS', T // SW))
BPS = SW // 128
CAP = 384
NBLK = CAP // 128
NE = 32
INW = 4360
EPS = 1e-6
LN32 = 3.4657359027997265
LN2 = 0.6931471805599453

C_AQ, C_AK, C_AV, C_MQ, C_MK, C_MV, C_MO, C_MI, C_GA, C_GB = 0, 512, 640, 768, 1024, 1280, 1792, 2304, 2312, 3336


class Sched:
    ENG = ("pe", "act", "dve", "pool", "sp")
    N_DMA_SEM = 24
    PSUM_KEYS = frozenset(("T0", "A0", "A1", "SC", "PV0", "PV1", "U0", "U1"))

    def __init__(self, nc, es):
        self.nc = nc
        self.q = {k: [] for k in self.ENG}
        self.sem = {k: es.enter_context(nc.semaphore("s_" + k)) for k in self.ENG}
        self.cnt = {k: 0 for k in self.ENG}
        self.dsem = [es.enter_context(nc.semaphore("s_dma%d" % i)) for i in range(self.N_DMA_SEM)]
        self.dcnt = [0] * self.N_DMA_SEM
        self.dpool = {"sp": list(range(0, 14)), "pool": list(range(14, self.N_DMA_SEM))}
        self.dnext = {"sp": 0, "pool": 0}
        self.waited = {k: {} for k in self.ENG}
        self.last_w = {}
        self.readers = {}

    def _semh(self, key):
        return self.sem[key] if isinstance(key, str) else self.dsem[key]

    def _wait(self, engine, tok):
        key, val = tok
        if key == engine and engine == "pe":
            return
        if self.waited[engine].get(key, 0) >= val:
            return
        self.waited[engine][key] = val
        h = self._semh(key)
        self.q[engine].append(lambda e, h=h, val=val: e.wait_ge(h, val))

    def _deps(self, engine, reads, writes):
        best = {}

        def add(d):
            for k, v in d.items():
                if best.get(k, 0) < v:
                    best[k] = v
        for r in reads:
            add(self.last_w.get(r, {}))
        for w in writes:
            add(self.last_w.get(w, {}))
            add(self.readers.get(w, {}))
        for k, v in best.items():
            self._wait(engine, (k, v))

    def _commit(self, tok, reads, writes):
        k, v = tok
        for r in reads:
            d = self.readers.setdefault(r, {})
            if d.get(k, 0) < v:
                d[k] = v
        for w in writes:
            d = self.last_w.setdefault(w, {})
            if d.get(k, 0) < v:
                d[k] = v
            self.readers[w] = {}

    nops = 0
    maxops = int(os.environ.get("K_MAXOPS", 10 ** 9))

    def op(self, engine, fn, reads=(), writes=()):
        self.nops += 1
        if self.nops > self.maxops:
            return None
        pr = [r for r in reads if r in self.PSUM_KEYS]
        if pr:
            reads = [r for r in reads if r not in self.PSUM_KEYS]
            writes = list(writes) + pr
        self._deps(engine, reads, writes)
        self.cnt[engine] += 1
        tok = (engine, self.cnt[engine])
        h = self.sem[engine]
        self.q[engine].append(lambda e, fn=fn, h=h: fn(e).then_inc(h, 1))
        self._commit(tok, reads, writes)
        return tok

    def dma(self, queue, fn, reads=(), writes=()):
        self.nops += 1
        if self.nops > self.maxops:
            return None
        self._deps(queue, reads, writes)
        pl = self.dpool[queue]
        i = pl[self.dnext[queue]]
        self.dnext[queue] = (self.dnext[queue] + 1) % len(pl)
        if self.dcnt[i] > 0:
            self._wait(queue, (i, self.dcnt[i]))
        self.dcnt[i] += 16
        tok = (i, self.dcnt[i])
        h = self.dsem[i]
        self.q[queue].append(lambda e, fn=fn, h=h: fn(e).then_inc(h, 16))
        self._commit(tok, reads, writes)
        return tok

    def barrier(self):
        for e in self.ENG:
            for f in self.ENG:
                if f != e and self.cnt[f] > 0:
                    self._wait(e, (f, self.cnt[f]))
            for i in range(self.N_DMA_SEM):
                if self.dcnt[i] > 0:
                    self._wait(e, (i, self.dcnt[i]))

    def emit(self):
        eng = {"pe": "tensor", "act": "scalar", "dve": "vector", "pool": "gpsimd", "sp": "sync"}
        with self.nc.Block() as block:
            for k in self.ENG:
                def body(e, k=k):
                    for f in self.q[k]:
                        f(e)
                getattr(block, eng[k])(body)


def build_program(debug=False, stop_after_phase1=False):
    nc = bass.Bass("TRN2", target_bir_lowering=False)

    def din(name, shape, dt=F32):
        return nc.dram_tensor(name, list(shape), dt, kind="ExternalInput").ap()

    scratch_kind = "ExternalOutput" if debug else "Internal"

    def dscr(name, shape, dt):
        return nc.dram_tensor(name, list(shape), dt, kind=scratch_kind).ap()

    x_d = din("x", [T, D])
    w_in_d = din("w_in", [D, INW])
    wgab_d = din("w_gab", [16, 128, 8, 128])
    wao_d = din("w_attn_o", [512, D])
    wmo_d = din("w_mlstm_o", [512, D])
    wout_d = din("w_out", [D, D])
    wr_d = din("w_rt", [D, 36])
    wg_d = din("w_gate", [NE, D, 512])
    wu_d = din("w_up", [NE, D, 512])
    wd_d = din("w_down", [NE, 512, D])
    nmw_d = din("norm_mix_w", [1, D])
    nfw_d = din("norm_ffn_w", [1, D])
    nlw_d = din("norm_final_w", [1, D])
    mnw_d = din("mlstm_norm_w", [1, 512])
    cw_d = din("conv_wt", [128, 4, 4])
    cb_d = din("conv_bt", [128, 4])
    bg_d = din("b_gates", [1, 8])
    sk_d = din("attn_sinks", [1, 8])
    brt_d = din("b_rt", [1, 36])
    identb_d = din("ident_bf", [128, 128], BF16)
    identf_d = din("ident_f", [128, 128])
    maskb_d = din("maskb", [128, 3, 128], BF16)
    cmask8_d = din("cmask8", [128, 128])
    trif_d = din("tri_f", [128, 128])
    onesf_d = din("ones_f", [128, 128])
    trisb_d = din("tris_bf", [128, 128], BF16)
    onesb_d = din("ones_bf", [128, 128], BF16)
    eoff_d = din("eoff", [1, NE])
    out_d = nc.dram_tensor("out", [T, D], F32, kind="ExternalOutput").ap()

    x2_d = dscr("x2_s", [T, D], F32)
    h2_d = dscr("h2_s", [T, D], BF16)
    xs_d = dscr("xs_s", [NE * CAP, D], BF16)
    ys_d = dscr("ys_s", [NE * CAP, D], F32)
    if debug:
        lg_d = nc.dram_tensor("lg_s", [128, NT, 36], F32, kind="ExternalOutput").ap()
        dst_d = nc.dram_tensor("dst_s", [128, 2, NT], I32, kind="ExternalOutput").ap()
        wts_d = nc.dram_tensor("wts_s", [128, 2, NT], F32, kind="ExternalOutput").ap()
        ya_dbg = nc.dram_tensor("ya_s", [T, 512], BF16, kind="ExternalOutput").ap()
        ym_dbg = nc.dram_tensor("ym_s", [T, 512], BF16, kind="ExternalOutput").ap()

    with ExitStack() as es:
        def sb(stack, name, shape, dt):
            return stack.enter_context(nc.sbuf_tensor("sb_" + name, list(shape), dt))

        def ps(name, shape, dt):
            return es.enter_context(nc.psum_tensor("ps_" + name, list(shape), dt))

        S = Sched(nc, es)

        T0 = ps("T0", [128, 8, 128], BF16)
        A0 = ps("A0", [128, 512], F32)
        A1 = ps("A1", [128, 512], F32)
        SC = ps("SC", [128, 512], F32)
        SC3 = SC[:].rearrange("p (a q) -> p a q", a=4)
        PV0 = ps("PV0", [128, 512], F32)
        PV1 = ps("PV1", [128, 512], F32)
        U0 = ps("U0", [128, 512], F32)
        U1 = ps("U1", [128, 512], F32)
        PV = [PV0, PV1]
        UU = [U0, U1]
        AA = [A0, A1]
        _bk = {"U0": (U0, "U0"), "PV1": (PV1, "PV1"), "SC": (SC, "SC"), "A0": (A0, "A0"), "PV0": (PV0, "PV0"),
               "A1": (A1, "A1"), "U1": (U1, "U1")}
        BANKS_AD = [_bk[k_] for k_ in os.environ.get("K_BANKS", "U0,PV1").split(",")]
        BANKS_C = [(A0, "A0"), (A1, "A1"), (SC, "SC"), (PV0, "PV0"), (PV1, "PV1"), (U0, "U0"), (U1, "U1")]
        bank_i = [0, 0]

        def nextbank():
            b_ = BANKS_AD[bank_i[0] % len(BANKS_AD)]
            bank_i[0] += 1
            return b_

        def nextbankC():
            b_ = BANKS_C[bank_i[1] % len(BANKS_C)]
            bank_i[1] += 1
            return b_

        identb = sb(es, "identb", [128, 128], BF16)
        identf = sb(es, "identf", [128, 128], F32)
        onesb = sb(es, "onesb", [128, 128], BF16)
        trisb = sb(es, "trisb", [128, 128], BF16)
        epsb = sb(es, "epsb", [128, 1], F32)
        lg_all = sb(es, "lg_all", [128, NT, 36], F32)

        def ld(dst, src, key, q="sp"):
            S.dma(q, lambda e: e.dma_start(out=dst, in_=src), writes=[key])

        ld(identb[:], identb_d, "identb")
        ld(identf[:], identf_d, "identf")
        ld(onesb[:], onesb_d, "onesb")
        ld(trisb[:], trisb_d, "trisb")
        S.op("pool", lambda e: e.memset(epsb[:], EPS), writes=["epsb"])
        lnb = sb(es, "lnb", [128, 1], F32)
        S.op("pool", lambda e: e.memset(lnb[:], -LN32), writes=["lnb"])
        neghalf = sb(es, "neghalf", [128, 4], F32)
        S.op("pool", lambda e: e.memset(neghalf[:], -0.5), writes=["neghalf"])

        def rsqrt_pool(out, in_, kin, kout, add_eps=True, n=1):
            if add_eps:
                S.op("pool", lambda e, in_=in_: e.tensor_scalar(out=out, in0=in_, scalar1=EPS, scalar2=None, op0=ALU.add),
                     reads=[kin], writes=[kout])
                in_, kin = out, kout
            S.op("pool", lambda e, in_=in_: e.tensor_tensor(out=out, in0=in_, in1=neghalf[:, 0:n], op=ALU.pow),
                 reads=[kin, "neghalf"], writes=[kout])
        S.op("pool", lambda e: e.memset(lg_all[:], 0.0), writes=["lg_all"])
        zt = sb(es, "zt", [128, D], BF16)
        S.op("pool", lambda e: e.memset(zt[:], 0.0), writes=["zt"])

        def zero_fill(ex_):
            if stop_after_phase1:
                return
            S.dma("sp", lambda e: e.dma_start(
                out=xs_d[ex_ * CAP:(ex_ + 1) * CAP, :].rearrange("(b p) d -> p b d", p=128),
                in_=zt[:].unsqueeze(1).to_broadcast([128, NBLK, D])), reads=["zt"], writes=["xz%d" % ex_])

        with ExitStack() as e1:
            w_in = sb(e1, "w_in", [128, 8, C_GA], BF16)
            NG = 4
            wgr = [sb(e1, "wgr%d" % i, [128, 8, 128], BF16) for i in range(NG)]
            wkz = sb(e1, "wkz", [128, 8, 4, 128], BF16)
            wao = sb(e1, "wao", [128, 4, D], BF16)
            wmo = sb(e1, "wmo", [128, 4, D], BF16)
            wout = sb(e1, "wout", [128, 8, D], BF16)
            wr = sb(e1, "wr", [128, 8, 36], F32)
            nmw = sb(e1, "nmw", [128, D], F32)
            nfw = sb(e1, "nfw", [128, D], F32)
            mnw = sb(e1, "mnw", [128, 512], F32)
            cw = sb(e1, "cw", [128, 4, 4], F32)
            cb = sb(e1, "cb", [128, 4], F32)
            bg = sb(e1, "bg", [128, 8], F32)
            esink = sb(e1, "esink", [128, 8], F32)
            maskb = sb(e1, "maskb", [128, 3, 128], BF16)
            cmask8 = sb(e1, "cmask8", [128, 128], F32)
            trif = sb(e1, "trif", [128, 128], F32)
            onesf = sb(e1, "onesf", [128, 128], F32)

            xt = [sb(e1, "xt%d" % i, [128, D], F32) for i in range(2 * BPS)]
            junk = sb(e1, "junk", [128, D], BF16)
            ss = sb(e1, "ss", [128, 1], F32)
            rstd = sb(e1, "rstd", [128, 1], F32)
            hb = sb(e1, "hb", [128, D], BF16)
            hT2 = [sb(e1, "hT%d" % i, [128, 8, SW], BF16) for i in range(2)]
            qT2 = [sb(e1, "qT%d" % i, [128, 4, SW], BF16) for i in range(2)]
            kTz2 = [sb(e1, "kTz%d" % i, [128, 4, 128 + SW], BF16) for i in range(2)]
            Vr2 = [sb(e1, "Vr%d" % i, [128, BPS + 1, 2, 65], BF16) for i in range(2)]
            pre2 = [sb(e1, "pre%d" % i, [128, 4, 3 + SW], F32) for i in range(2)]
            cacc = sb(e1, "cacc", [128, SW], F32)
            qmT = sb(e1, "qmT", [128, 2, SW], BF16)
            kmTz = sb(e1, "kmTz", [128, 2, 2, SW], BF16)
            kmT = sb(e1, "kmT", [128, 2, SW], BF16)
            ktok = sb(e1, "ktok", [128, 2, 128], BF16)
            vm2 = [[sb(e1, "vm%d_%d" % (i, p_), [128, 512], BF16) for i in range(BPS)] for p_ in range(2)]
            so2 = [[sb(e1, "so%d_%d" % (i, p_), [128, 512], F32) for i in range(BPS)] for p_ in range(2)]
            gtt2 = [sb(e1, "gtt%d" % p_, [128, BPS, 8], F32) for p_ in range(2)]
            nlfa = sb(e1, "nlfa", [128, BPS, 4], F32)
            lsu = sb(e1, "lsu", [128, BPS, 4], F32)
            lsz = sb(e1, "lsz", [128, BPS, 4], F32)
            lsz2 = sb(e1, "lsz2", [128, BPS, 4], F32)
            lsq = sb(e1, "lsq", [128, BPS, 4], F32)
            gtmp = sb(e1, "gtmp", [128, 16], F32)
            ex = [sb(e1, "ex%d" % i, [128, 16], F32) for i in range(2)]
            vp = sb(e1, "vp", [128, 4, 129], BF16)
            ATb = sb(e1, "ATb", [128, 4, 128], BF16)
            Yst = sb(e1, "Yst", [128, 4, 129], F32)
            Czb = sb(e1, "Czb", [128, 4, 129], BF16)
            PT = [sb(e1, "PT%d" % i, [128, 4, 128], BF16) for i in range(2)]
            den = sb(e1, "den", [128, 8], F32)
            ya = sb(e1, "ya", [128, 512], BF16)
            ym = sb(e1, "ym", [128, 512], BF16)
            yaT2 = [sb(e1, "yaT%d" % i, [128, 4, SW], BF16) for i in range(2)]
            ymT2 = [sb(e1, "ymT%d" % i, [128, 4, SW], BF16) for i in range(2)]
            absd = sb(e1, "absd", [128, 4], F32)
            ssn = sb(e1, "ssn", [128, 4], F32)
            dm = sb(e1, "dm", [128, 4], F32)
            d2 = sb(e1, "d2", [128, 4], F32)
            sc4 = sb(e1, "sc4", [128, 4], F32)
            sgm = [sb(e1, "sgm%d" % i, [128, SW], F32) for i in range(2)]
            tm = [sb(e1, "tm%d" % i, [128, SW], F32) for i in range(2)]
            mixT = sb(e1, "mixT", [128, 8, SW], BF16)
            h2f = sb(e1, "h2f", [128, D], F32)
            h2b = sb(e1, "h2b", [128, D], BF16)
            h2Th = sb(e1, "h2Th", [128, 8, 128], BF16)
            h2Tl = sb(e1, "h2Tl", [128, 8, 128], BF16)
            wrh = sb(e1, "wrh", [128, 8, 36], BF16)
            wrl = sb(e1, "wrl", [128, 8, 36], BF16)
            ss2 = sb(e1, "ss2", [128, 1], F32)
            rstd2 = sb(e1, "rstd2", [128, 1], F32)

            w_in_v = w_in_d.rearrange("(kc p) n -> p kc n", p=128)
            for c0 in range(0, C_GA, 1024):
                c1 = min(C_GA, c0 + 1024)
                S.dma("pool", lambda e, c0=c0, c1=c1: e.dma_start(out=w_in[:, :, c0:c1], in_=w_in_v[:, :, c0:c1]),
                      writes=["w_in%d" % (c0 // 1024)])
            S.op("pool", lambda e: e.memset(wkz[:], 0.0), writes=["wkz"])
            for kv in range(2):
                for hh in range(2):
                    S.op("dve", lambda e, kv=kv, hh=hh: e.tensor_copy(
                        out=wkz[:, :, kv * 2 + hh, hh * 64:hh * 64 + 64],
                        in_=w_in[:, :, C_AK + kv * 64:C_AK + kv * 64 + 64]), reads=["w_in0", "wkz"], writes=["wkz"])
            S.dma("pool", lambda e: e.dma_start(out=wao[:], in_=wao_d.rearrange("(kc p) n -> p kc n", p=128)),
                  writes=["wao"])
            S.dma("pool", lambda e: e.dma_start(out=wmo[:], in_=wmo_d.rearrange("(kc p) n -> p kc n", p=128)),
                  writes=["wmo"])
            S.dma("pool", lambda e: e.dma_start(out=wout[:], in_=wout_d.rearrange("(kc p) n -> p kc n", p=128)),
                  writes=["wout"])
            ld(wr[:], wr_d.rearrange("(kc p) n -> p kc n", p=128), "wr")
            S.op("dve", lambda e: e.tensor_copy(out=wrh[:], in_=wr[:]), reads=["wr"], writes=["wrh"])
            S.op("dve", lambda e: e.tensor_tensor(out=wrl[:], in0=wr[:], in1=wrh[:], op=ALU.subtract),
                 reads=["wr", "wrh"], writes=["wrl"])
            ld(nmw[:], nmw_d.partition_broadcast(128), "nmw")
            ld(nfw[:], nfw_d.partition_broadcast(128), "nfw")
            ld(mnw[:], mnw_d.partition_broadcast(128), "mnw")
            ld(cw[:], cw_d, "cw")
            ld(cb[:], cb_d, "cb")
            ld(bg[:], bg_d.partition_broadcast(128), "bg")
            ld(esink[:], sk_d.partition_broadcast(128), "esink")
            ld(maskb[:], maskb_d, "maskb")
            ld(cmask8[:], cmask8_d, "cmask8")
            ld(trif[:], trif_d, "trif")
            ld(onesf[:], onesf_d, "onesf")
            S.op("act", lambda e: e.activation(out=esink[:], in_=esink[:], func=AF.Exp, bias=LN2, scale=1.0),
                 reads=["esink"], writes=["esink"])
            S.op("dve", lambda e: e.tensor_scalar(out=mnw[:], in0=mnw[:], scalar1=0.25, scalar2=None, op0=ALU.mult),
                 reads=["mnw"], writes=["mnw"])
            for p_ in range(2):
                S.op("pool", lambda e, p_=p_: e.memset(kTz2[p_][:], 0.0), writes=["kTz%d" % p_])
                S.op("pool", lambda e, p_=p_: e.memset(Vr2[p_][:], 0.0), writes=["Vr%d" % p_])
                S.op("pool", lambda e, p_=p_: e.memset(Vr2[p_][:, :, :, 64:65], 2.0), reads=["Vr%d" % p_], writes=["Vr%d" % p_])
                S.op("pool", lambda e, p_=p_: e.memset(pre2[p_][:], 0.0), writes=["pre%d" % p_])
            S.op("pool", lambda e: e.memset(kmTz[:], 0.0), writes=["kmTz"])
            S.op("pool", lambda e: e.memset(Yst[:], 0.0), writes=["Yst"])
            S.op("pool", lambda e: e.memset(Czb[:], 0.0), writes=["Czb"])
            S.op("pool", lambda e: e.memset(ex[1][:], 1.0), writes=["ex1"])

            def wk(c0, n):
                return ["w_in%d" % c for c in range(c0 // 1024, (c0 + n - 1) // 1024 + 1)]

            def mm(out, lhsT, rhs, st, sp_, R, W):
                S.op("pe", lambda e: e.matmul(out, lhsT=lhsT, rhs=rhs, start=st, stop=sp_), reads=R, writes=W)

            def tr(out, in_, idt, R, W):
                S.op("pe", lambda e: e.transpose(out=out, in_=in_, identity=idt), reads=R, writes=W)

            def xi(s, i):
                return (s % 2) * BPS + i

            def load_x(s, i):
                t0 = s * SW + i * 128
                S.dma("sp", lambda e: e.dma_start(out=xt[xi(s, i)][:], in_=x_d[t0:t0 + 128, :]),
                      writes=["xt%d" % xi(s, i)])

            for s_ in range(min(2, NS)):
                for i in range(BPS):
                    load_x(s_, i)

            evac_flip = [0]

            def evac(out, in_, R, W, scale=None):
                evac_flip[0] ^= 1
                if evac_flip[0]:
                    S.op("act", lambda e: e.copy(out=out, in_=in_), reads=R, writes=W)
                else:
                    S.op("dve", lambda e: e.tensor_copy(out=out, in_=in_), reads=R, writes=W)

            gab_next = [0]
            N_GAB = NS * 16

            def gab_prefetch():
                t = gab_next[0]
                if t >= N_GAB:
                    return
                gab_next[0] += 1
                mm_ = (t % 16) // 2 + 8 * (t % 2)
                S.dma("pool", lambda e: e.dma_start(out=wgr[t % NG][:], in_=wgab_d[mm_]), writes=["wgr%d" % (t % NG)])

            def gab_tile(s_, mt):
                t = 16 * s_ + 2 * (mt % 8) + (1 if mt >= 8 else 0)
                assert t < gab_next[0], "ga/gb tile used before it was prefetched"
                assert t >= gab_next[0] - NG
                return wgr[t % NG], "wgr%d" % (t % NG)

            for _ in range(NG):
                gab_prefetch()

            def gA(s):
                p_ = s % 2
                hT, qT, kTz, Vr, pre, vm, so, gtt = hT2[p_], qT2[p_], kTz2[p_], Vr2[p_], pre2[p_], vm2[p_], so2[p_], gtt2[p_]
                gt = [gtt[:, i, :] for i in range(BPS)]
                kHT, kQT, kKT, kVR, kPRE = "hT%d" % p_, "qT%d" % p_, "kTz%d" % p_, "Vr%d" % p_, "pre%d" % p_
                kVM, kSO, kGT = "vm%d_" + str(p_), "so%d_" + str(p_), "gt%d_" + str(p_)
                for i in range(BPS):
                    xk = "xt%d" % xi(s, i)
                    S.op("act", lambda e, i=i: e.activation(out=hb[:], in_=xt[xi(s, i)][:], func=AF.Square,
                                                            scale=1.0 / 32, accum_out=ss[:]),
                         reads=[xk], writes=["hb", "ss"])
                    rsqrt_pool(rstd[:], ss[:], "ss", "rstd")
                    S.op("dve", lambda e, i=i: e.scalar_tensor_tensor(out=hb[:], in0=xt[xi(s, i)][:], scalar=rstd[:], in1=nmw[:],
                                                                      op0=ALU.mult, op1=ALU.mult),
                         reads=[xk, "rstd", "nmw"], writes=["hb"])
                    yield
                    for kc in range(8):
                        tr(T0[:, kc, :], hb[:, kc * 128:(kc + 1) * 128], identb[:], ["hb", "identb"], ["T0"])
                    evac(hT[:, :, i * 128:(i + 1) * 128], T0[:], ["T0"], [kHT])
                    yield

                if s > 0:
                    q_ = 1 - p_
                    S.op("pool", lambda e: e.tensor_copy(out=kTz[:, :, 0:128], in_=kTz2[q_][:, :, SW:SW + 128]),
                         reads=["kTz%d" % q_], writes=[kKT])
                    S.op("pool", lambda e: e.tensor_copy(out=Vr[:, 0, :, :], in_=Vr2[q_][:, BPS, :, :]),
                         reads=["Vr%d" % q_], writes=[kVR])
                    S.op("pool", lambda e: e.tensor_copy(out=pre[:, :, 0:3], in_=pre2[q_][:, :, SW:SW + 3]),
                         reads=["pre%d" % q_], writes=[kPRE])
                for m in range(12):
                    bt_, ak = nextbank()
                    acc = bt_[:, 0:SW]
                    for kc in range(8):
                        if m < 4:
                            lhsT = w_in[:, kc, C_AQ + m * 128:C_AQ + (m + 1) * 128]
                            wkk = wk(C_AQ + m * 128, 128)
                        elif m < 8:
                            lhsT = wkz[:, kc, m - 4, :]
                            wkk = ["wkz"]
                        else:
                            c0 = C_MQ + (m - 8) * 128
                            lhsT = w_in[:, kc, c0:c0 + 128]
                            wkk = wk(c0, 128)
                        mm(acc, lhsT, hT[:, kc, :], kc == 0, kc == 7, wkk + [kHT], [ak])
                    if m < 4:
                        evac(qT[:, m, :], acc, [ak], [kQT])
                    elif m < 8:
                        evac(kTz[:, m - 4, 128:128 + SW], acc, [ak], [kKT])
                    else:
                        evac(pre[:, m - 8, 3:3 + SW], acc, [ak], [kPRE])
                    yield

                for b in range(BPS):
                    tsl = slice(b * 128, (b + 1) * 128)
                    bv, bvk = nextbank()
                    for kc in range(8):
                        mm(bv[:, 0:128], hT[:, kc, tsl], w_in[:, kc, C_AV:C_AV + 128], kc == 0, kc == 7,
                           [kHT] + wk(C_AV, 128), [bvk])
                    for kc in range(8):
                        mm(bv[:, 128:136], hT[:, kc, tsl], w_in[:, kc, C_MI:C_MI + 8], kc == 0, kc == 7,
                           [kHT] + wk(C_MI, 8), [bvk])
                    S.op("act", lambda e, b=b, bv=bv: e.copy(out=Vr[:, b + 1, :, 0:64],
                                                             in_=bv[:, 0:128].rearrange("p (k d) -> p k d", k=2)),
                         reads=[bvk], writes=[kVR])
                    S.op("dve", lambda e, b=b, bv=bv: e.tensor_tensor(out=gt[b], in0=bv[:, 128:136], in1=bg[:], op=ALU.add),
                         reads=[bvk, "bg"], writes=[kGT % b])
                    yield
                    bm, bmk = nextbank()
                    for kc in range(8):
                        mm(bm[:, :], hT[:, kc, tsl], w_in[:, kc, C_MV:C_MV + 512], kc == 0, kc == 7,
                           [kHT] + wk(C_MV, 512), [bmk])
                    S.op("dve", lambda e, b=b, bm=bm: e.tensor_copy(out=vm[b][:], in_=bm[:]), reads=[bmk], writes=[kVM % b])
                    yield
                    bo, bok = nextbank()
                    for kc in range(8):
                        mm(bo[:, :], hT[:, kc, tsl], w_in[:, kc, C_MO:C_MO + 512], kc == 0, kc == 7,
                           [kHT] + wk(C_MO, 512), [bok])
                    S.op("act", lambda e, b=b, bo=bo: e.activation(out=so[b][:], in_=bo[:], func=AF.Tanh, scale=0.5),
                         reads=[bok], writes=[kSO % b])
                    S.op("dve", lambda e, b=b: e.scalar_tensor_tensor(out=so[b][:], in0=so[b][:], scalar=1.0, in1=mnw[:],
                                                                      op0=ALU.add, op1=ALU.mult),
                         reads=[kSO % b, "mnw"], writes=[kSO % b])

                    yield

            def gG(s):
                p_ = s % 2
                hT, qT, kTz, Vr, pre, vm, so, gtt = hT2[p_], qT2[p_], kTz2[p_], Vr2[p_], pre2[p_], vm2[p_], so2[p_], gtt2[p_]
                gt = [gtt[:, i, :] for i in range(BPS)]
                kHT, kQT, kKT, kVR, kPRE = "hT%d" % p_, "qT%d" % p_, "kTz%d" % p_, "Vr%d" % p_, "pre%d" % p_
                kVM, kSO, kGT = "vm%d_" + str(p_), "so%d_" + str(p_), "gt%d_" + str(p_)
                gks = [kGT % b for b in range(BPS)]
                xf = gtt[:, :, 4:8]
                S.op("act", lambda e: e.activation(out=lsu[:], in_=xf, func=AF.Abs), reads=gks, writes=["lsu"])
                S.op("act", lambda e: e.activation(out=lsu[:], in_=lsu[:], func=AF.Exp, scale=-1.0),
                     reads=["lsu"], writes=["lsu"])
                S.op("dve", lambda e: e.tensor_scalar(out=lsz[:], in0=lsu[:], scalar1=2.0, scalar2=None, op0=ALU.add),
                     reads=["lsu"], writes=["lsz"])
                S.op("dve", lambda e: e.reciprocal(out=lsz[:], in_=lsz[:]), reads=["lsz"], writes=["lsz"])
                S.op("dve", lambda e: e.tensor_tensor(out=lsz[:], in0=lsz[:], in1=lsu[:], op=ALU.mult),
                     reads=["lsz", "lsu"], writes=["lsz"])
                S.op("dve", lambda e: e.tensor_tensor(out=lsz2[:], in0=lsz[:], in1=lsz[:], op=ALU.mult),
                     reads=["lsz"], writes=["lsz2"])
                S.op("dve", lambda e: e.tensor_scalar(out=lsq[:], in0=lsz2[:], scalar1=1.0 / 11, scalar2=None,
                                                      op0=ALU.mult), reads=["lsz2"], writes=["lsq"])
                for cst in (1.0 / 9, 1.0 / 7, 1.0 / 5, 1.0 / 3):
                    S.op("dve", lambda e, cst=cst: e.scalar_tensor_tensor(out=lsq[:], in0=lsq[:], scalar=cst, in1=lsz2[:],
                                                                          op0=ALU.add, op1=ALU.mult),
                         reads=["lsq", "lsz2"], writes=["lsq"])
                S.op("dve", lambda e: e.scalar_tensor_tensor(out=lsq[:], in0=lsq[:], scalar=1.0, in1=lsz[:],
                                                             op0=ALU.add, op1=ALU.mult),
                     reads=["lsq", "lsz"], writes=["lsq"])
                S.op("dve", lambda e: e.tensor_scalar(out=lsu[:], in0=xf, scalar1=0.0, scalar2=None, op0=ALU.min),
                     reads=gks + ["lsu"], writes=["lsu"])
                S.op("dve", lambda e: e.scalar_tensor_tensor(out=nlfa[:], in0=lsq[:], scalar=2.0, in1=lsu[:],
                                                             op0=ALU.mult, op1=ALU.subtract),
                     reads=["lsq", "lsu"], writes=["nlfa"])
                yield

                for t4 in range(4):
                    S.op("dve", lambda e, t4=t4: e.tensor_scalar(out=cacc[:], in0=pre[:, t4, 0:SW],
                                                                 scalar1=cw[:, t4, 0:1], scalar2=cb[:, t4:t4 + 1],
                                                                 op0=ALU.mult, op1=ALU.add),
                         reads=[kPRE, "cw", "cb"], writes=["cacc"])
                    for j in range(1, 4):
                        S.op("dve", lambda e, t4=t4, j=j: e.scalar_tensor_tensor(
                            out=cacc[:], in0=pre[:, t4, j:j + SW], scalar=cw[:, t4, j:j + 1],
                            in1=cacc[:], op0=ALU.mult, op1=ALU.add),
                            reads=[kPRE, "cw", "cacc"], writes=["cacc"])
                    S.op("act", lambda e: e.activation(out=tm[0][:], in_=cacc[:], func=AF.Tanh, scale=0.5),
                         reads=["cacc"], writes=["tm0"])
                    if t4 < 2:
                        S.op("dve", lambda e, t4=t4: e.scalar_tensor_tensor(out=qmT[:, t4, :], in0=tm[0][:], scalar=1.0,
                                                                            in1=cacc[:], op0=ALU.add, op1=ALU.mult),
                             reads=["tm0", "cacc"], writes=["qmT"])
                    else:
                        S.op("dve", lambda e, t4=t4: e.scalar_tensor_tensor(out=kmT[:, t4 - 2, :], in0=tm[0][:], scalar=1.0,
                                                                            in1=cacc[:], op0=ALU.add, op1=ALU.mult),
                             reads=["tm0", "cacc"], writes=["kmT"])
                        for hh in range(2):
                            ps_ = slice(hh * 64, hh * 64 + 64)
                            S.op("pool", lambda e, t4=t4, hh=hh, ps_=ps_: e.tensor_copy(
                                out=kmTz[ps_, t4 - 2, hh, :], in_=kmT[ps_, t4 - 2, :]),
                                reads=["kmT"], writes=["kmTz"])
                    yield

            def gX(s):
                p_ = s % 2
                yaT, ymT, kYA, kYM = yaT2[p_], ymT2[p_], "yaT%d" % p_, "ymT%d" % p_
                hT, qT, kTz, Vr, pre, vm, so, gtt = hT2[p_], qT2[p_], kTz2[p_], Vr2[p_], pre2[p_], vm2[p_], so2[p_], gtt2[p_]
                gt = [gtt[:, i, :] for i in range(BPS)]
                kHT, kQT, kKT, kVR, kPRE = "hT%d" % p_, "qT%d" % p_, "kTz%d" % p_, "Vr%d" % p_, "pre%d" % p_
                kVM, kSO, kGT = "vm%d_" + str(p_), "so%d_" + str(p_), "gt%d_" + str(p_)
                sc3 = SC[:].rearrange("p (a q) -> p a q", a=4)
                pv3 = PV0[:, 0:260].rearrange("p (h d) -> p h d", h=4)
                for b in range(BPS):
                    gb = s * BPS + b
                    qsl = slice(b * 128, (b + 1) * 128)
                    for g in range(2):
                        for j in (2 * g, 2 * g + 1):
                            kv = j // 2
                            ptk = "PT%d" % (j % 2)
                            ptb = PT[j % 2]
                            for hh in range(2):
                                for kt in range(2):
                                    o = sc3[:, hh * 2 + kt, :]
                                    mm(o, kTz[:, kv * 2 + hh, (b + kt) * 128:(b + kt + 1) * 128], qT[:, j, qsl],
                                       True, False, [kKT, kQT], ["SC"])
                                    mi_ = 1 if kt == 1 else (2 if gb == 0 else 0)
                                    mm(o, identb[:], maskb[:, mi_, :], False, True, ["identb", "maskb"], ["SC"])
                            S.op("act", lambda e, ptb=ptb: e.activation(out=ptb[:], in_=sc3, func=AF.Exp, scale=0.125),
                                 reads=["SC"], writes=[ptk])
                            yield
                            for hh in range(2):
                                head = 2 * j + hh
                                o = PV0[:, (head % 4) * 65:(head % 4) * 65 + 65]
                                for kt in range(2):
                                    mm(o, ptb[:, hh * 2 + kt, :], Vr[:, b + kt, kv, :], kt == 0, kt == 1,
                                       [ptk, kVR], ["PV0"])
                            yield
                        S.op("dve", lambda e, g=g: e.tensor_tensor(
                            out=den[:, 4 * g:4 * g + 4], in0=pv3[:, :, 64], in1=esink[:, 4 * g:4 * g + 4], op=ALU.add),
                            reads=["PV0", "esink"], writes=["den"])
                        S.op("dve", lambda e, g=g: e.reciprocal(out=den[:, 4 * g:4 * g + 4], in_=den[:, 4 * g:4 * g + 4]),
                             reads=["den"], writes=["den"])
                        S.op("dve", lambda e, g=g: e.tensor_tensor(
                            out=ya[:, 256 * g:256 * g + 256].rearrange("p (h d) -> p h d", h=4),
                            in0=pv3[:, :, 0:64],
                            in1=den[:, 4 * g:4 * g + 4].unsqueeze(2).to_broadcast([128, 4, 64]), op=ALU.mult),
                            reads=["PV0", "den"], writes=["ya"])
                    yield
                    for c in range(4):
                        tr(T0[:, c, :], ya[:, c * 128:(c + 1) * 128], identb[:], ["ya", "identb"], ["T0"])
                    evac(yaT[:, :, qsl], T0[:, 0:4, :], ["T0"], [kYA])
                    yield
                    if debug:
                        t0 = gb * 128
                        S.dma("sp", lambda e, t0=t0: e.dma_start(out=ya_dbg[t0:t0 + 128, :], in_=ya[:]), reads=["ya"])

            A03 = A0[:].rearrange("p (a q) -> p a q", a=4)
            sqf = junk[:].bitcast(F32).rearrange("p (h d) -> p h d", h=4)
            ND = [A1, U1]
            NDK = ["A1", "U1"]

            def gY(s):
                p_ = s % 2
                yaT, ymT, kYA, kYM = yaT2[p_], ymT2[p_], "yaT%d" % p_, "ymT%d" % p_
                hT, qT, kTz, Vr, pre, vm, so, gtt = hT2[p_], qT2[p_], kTz2[p_], Vr2[p_], pre2[p_], vm2[p_], so2[p_], gtt2[p_]
                gt = [gtt[:, i, :] for i in range(BPS)]
                kHT, kQT, kKT, kVR, kPRE = "hT%d" % p_, "qT%d" % p_, "kTz%d" % p_, "Vr%d" % p_, "pre%d" % p_
                kVM, kSO, kGT = "vm%d_" + str(p_), "so%d_" + str(p_), "gt%d_" + str(p_)
                for b in range(BPS):
                    gb = s * BPS + b
                    csl = slice(b * 128, (b + 1) * 128)
                    gk = kGT % b
                    exc = ex[gb % 2]
                    exk = "ex%d" % (gb % 2)
                    exp_ = ex[(gb + 1) % 2]
                    exkp = "ex%d" % ((gb + 1) % 2)
                    for t2 in range(2):
                        tr(T0[:, t2, :], kmT[:, t2, csl], identb[:], ["kmT", "identb"], ["T0"])
                    S.op("dve", lambda e: e.tensor_copy(out=ktok[:], in_=T0[:, 0:2, :]), reads=["T0"], writes=["ktok"])
                    mm(A1[:, 0:4], trif[:], nlfa[:, b, :], True, True, ["trif", "nlfa"], ["A1"])
                    mm(A1[:, 4:8], onesf[:], nlfa[:, b, :], True, True, ["onesf", "nlfa"], ["A1"])
                    S.op("dve", lambda e, b=b: e.tensor_tensor(out=gtmp[:, 0:4], in0=A1[:, 0:4], in1=gt[b][:, 0:4],
                                                               op=ALU.add), reads=["A1", gk], writes=["gtmp"])
                    S.op("act", lambda e, exc=exc: e.activation(out=exc[:, 4:8], in_=A1[:, 0:4], func=AF.Exp),
                         reads=["A1"], writes=[exk])
                    S.op("act", lambda e, exc=exc: e.activation(out=exc[:, 8:12], in_=A1[:, 4:8], func=AF.Exp,
                                                                bias=lnb[:], scale=-1.0),
                         reads=["A1", "lnb"], writes=[exk])
                    S.op("act", lambda e, exc=exc: e.activation(out=exc[:, 12:16], in_=A1[:, 4:8], func=AF.Exp, scale=-1.0),
                         reads=["A1"], writes=[exk])
                    S.op("act", lambda e, exc=exc: e.activation(out=exc[:, 0:4], in_=gtmp[:, 0:4], func=AF.Exp),
                         reads=["gtmp"], writes=[exk])
                    yield
                    for h in range(4):
                        mm(A03[:, h, :], kmTz[:, h // 2, h % 2, csl], qmT[:, h // 2, csl], True, True,
                           ["kmTz", "qmT"], ["A0"])
                    S.op("dve", lambda e: e.tensor_tensor(out=ATb[:], in0=A03,
                                                          in1=cmask8[:].unsqueeze(1).to_broadcast([128, 4, 128]),
                                                          op=ALU.mult), reads=["A0", "cmask8"], writes=["ATb"])
                    S.op("dve", lambda e, b=b, exc=exc: e.tensor_tensor(
                        out=vp[:, :, 0:128], in0=vm[b][:].rearrange("p (h d) -> p h d", h=4),
                        in1=exc[:, 0:4].unsqueeze(2).to_broadcast([128, 4, 128]), op=ALU.mult),
                        reads=[kVM % b, exk], writes=["vp"])
                    S.op("dve", lambda e, exc=exc: e.tensor_copy(out=vp[:, :, 128], in_=exc[:, 0:4]),
                         reads=[exk], writes=["vp"])
                    yield
                    for h in range(4):
                        g = h // 2
                        o = ND[g][:, (h % 2) * 129:(h % 2) * 129 + 129]
                        mm(o, qmT[:, h // 2, csl], Czb[:, h, :], True, False, ["qmT", "Czb"], [NDK[g]])
                        mm(o, ATb[:, h, :], vp[:, h, :], False, True, ["ATb", "vp"], [NDK[g]])
                    for g in range(2):
                        pv3 = ND[g][:, 0:258].rearrange("p (h d) -> p h d", h=2)
                        S.op("act", lambda e, g=g, pv3=pv3: e.activation(out=absd[:, 2 * g:2 * g + 2], in_=pv3[:, :, 128],
                                                                         func=AF.Abs),
                             reads=[NDK[g]], writes=["absd"])
                        S.op("act", lambda e, g=g, pv3=pv3: e.activation(
                            out=sqf[:, 2 * g:2 * g + 2, :], in_=pv3[:, :, 0:128], func=AF.Square, scale=128.0 ** -0.5),
                            reads=[NDK[g]], writes=["junk"])
                    yield
                    for h in range(4):
                        if h % 2 == 0:
                            for h2_ in (h, h + 1):
                                o = A0[:, (h2_ % 2) * 129:(h2_ % 2) * 129 + 129]
                                mm(o, ktok[:, h2_ // 2, :], vp[:, h2_, :], True, True, ["ktok", "vp"], ["A0"])
                        rs = slice((h % 2) * 64, (h % 2) * 64 + 64)
                        u = A0[rs, (h % 2) * 129:(h % 2) * 129 + 129]
                        S.op("dve", lambda e, h=h, rs=rs, u=u, exp_=exp_: e.scalar_tensor_tensor(
                            out=Yst[rs, h, :], in0=Yst[rs, h, :], scalar=exp_[rs, 12 + h:13 + h], in1=u,
                            op0=ALU.mult, op1=ALU.add), reads=["Yst", exkp, "A0"], writes=["Yst"])
                        S.op("act", lambda e, h=h, rs=rs, exc=exc: e.activation(
                            out=Czb[rs, h, :], in_=Yst[rs, h, :], func=AF.Copy, scale=exc[rs, 8 + h:9 + h]),
                            reads=["Yst", exk], writes=["Czb"])
                        if h == 1:
                            yield
                    S.op("dve", lambda e: e.tensor_reduce(out=ssn[:], in_=sqf, axis=AX.X, op=ALU.add),
                         reads=["junk"], writes=["ssn"])
                    S.op("dve", lambda e, exc=exc: e.tensor_tensor(out=dm[:], in0=absd[:], in1=exc[:, 4:8], op=ALU.max),
                         reads=["absd", exk], writes=["dm"])
                    S.op("dve", lambda e: e.tensor_tensor(out=d2[:], in0=dm[:], in1=dm[:], op=ALU.mult),
                         reads=["dm"], writes=["d2"])
                    S.op("dve", lambda e: e.scalar_tensor_tensor(out=d2[:], in0=d2[:], scalar=EPS, in1=ssn[:],
                                                                  op0=ALU.mult, op1=ALU.add),
                         reads=["d2", "ssn"], writes=["d2"])
                    rsqrt_pool(sc4[:], d2[:], "d2", "sc4", add_eps=False, n=4)
                    yield
                    for h in range(4):
                        g = h // 2
                        o = ND[g][:, (h % 2) * 129:(h % 2) * 129 + 128]
                        S.op("dve", lambda e, h=h, o=o, b=b: e.scalar_tensor_tensor(
                            out=ym[:, h * 128:(h + 1) * 128], in0=o, scalar=sc4[:, h:h + 1],
                            in1=so[b][:, h * 128:(h + 1) * 128], op0=ALU.mult, op1=ALU.mult),
                            reads=[NDK[g], "sc4", kSO % b], writes=["ym"])
                    yield
                    for c in range(4):
                        tr(T0[:, 4 + c, :], ym[:, c * 128:(c + 1) * 128], identb[:], ["ym", "identb"], ["T0"])
                    evac(ymT[:, :, csl], T0[:, 4:8, :], ["T0"], [kYM])
                    yield
                    if debug:
                        t0 = gb * 128
                        S.dma("sp", lambda e, t0=t0: e.dma_start(out=ym_dbg[t0:t0 + 128, :], in_=ym[:]), reads=["ym"])

            def gC(s):
                p_ = s % 2
                yaT, ymT, kYA, kYM = yaT2[p_], ymT2[p_], "yaT%d" % p_, "ymT%d" % p_
                hT, qT, kTz, Vr, pre, vm, so, gtt = hT2[p_], qT2[p_], kTz2[p_], Vr2[p_], pre2[p_], vm2[p_], so2[p_], gtt2[p_]
                gt = [gtt[:, i, :] for i in range(BPS)]
                kHT, kQT, kKT, kVR, kPRE = "hT%d" % p_, "qT%d" % p_, "kTz%d" % p_, "Vr%d" % p_, "pre%d" % p_
                kVM, kSO, kGT = "vm%d_" + str(p_), "so%d_" + str(p_), "gt%d_" + str(p_)
                for m in range(8):
                    msl = slice(m * 128, (m + 1) * 128)
                    bka, ka = nextbank()
                    bkb, kb = nextbank()
                    a0 = bka[:, 0:SW]
                    a1 = bka[:, SW:2 * SW]
                    b0 = bkb[:, 0:SW]
                    b1 = bkb[:, SW:2 * SW]
                    ta = gab_tile(s, m)
                    tb = gab_tile(s, 8 + m)
                    for kc in range(8):
                        mm(a0, ta[0][:, kc, :], hT[:, kc, :], kc == 0, kc == 7, [ta[1], kHT], [ka])
                    for kc in range(4):
                        mm(a1, wao[:, kc, msl], yaT[:, kc, :], kc == 0, kc == 3, ["wao", kYA], [ka])
                    for kc in range(8):
                        mm(b0, tb[0][:, kc, :], hT[:, kc, :], kc == 0, kc == 7, [tb[1], kHT], [kb])
                    for kc in range(4):
                        mm(b1, wmo[:, kc, msl], ymT[:, kc, :], kc == 0, kc == 3, ["wmo", kYM], [kb])
                    gab_prefetch()
                    gab_prefetch()
                    S.op("act", lambda e, a0=a0: e.activation(out=sgm[0][:], in_=a0, func=AF.Tanh, scale=0.5),
                         reads=[ka], writes=["sgm0"])
                    S.op("act", lambda e, b0=b0: e.activation(out=sgm[1][:], in_=b0, func=AF.Tanh, scale=0.5),
                         reads=[kb], writes=["sgm1"])
                    S.op("dve", lambda e, a1=a1: e.scalar_tensor_tensor(out=tm[0][:], in0=sgm[0][:], scalar=1.0, in1=a1,
                                                                        op0=ALU.add, op1=ALU.mult),
                         reads=[ka, "sgm0"], writes=["tm0"])
                    S.op("dve", lambda e, b1=b1: e.scalar_tensor_tensor(out=tm[1][:], in0=sgm[1][:], scalar=1.0, in1=b1,
                                                                        op0=ALU.add, op1=ALU.mult),
                         reads=[kb, "sgm1"], writes=["tm1"])
                    S.op("dve", lambda e, m=m: e.tensor_tensor(out=mixT[:, m, :], in0=tm[0][:], in1=tm[1][:], op=ALU.add),
                         reads=["tm0", "tm1"], writes=["mixT"])
                    yield

            def gD(s):
                for b in range(BPS):
                    gb = s * BPS + b
                    t0 = gb * 128
                    tsl = slice(b * 128, (b + 1) * 128)
                    for half in range(2):
                        bo_, bok_ = nextbank()
                        for kc in range(8):
                            mm(bo_[:], mixT[:, kc, tsl], wout[:, kc, half * 512:(half + 1) * 512], kc == 0, kc == 7,
                               ["mixT", "wout"], [bok_])
                        S.op("dve", lambda e, b=b, half=half, bo_=bo_: e.tensor_tensor(
                            out=xt[xi(s, b)][:, half * 512:(half + 1) * 512], in0=bo_[:],
                            in1=xt[xi(s, b)][:, half * 512:(half + 1) * 512], op=ALU.add),
                            reads=[bok_, "xt%d" % xi(s, b)], writes=["xt%d" % xi(s, b)])
                        yield
                    x2 = xt[xi(s, b)]
                    xk2 = "xt%d" % xi(s, b)
                    S.dma("sp", lambda e, t0=t0, x2=x2: e.dma_start(out=x2_d[t0:t0 + 128, :], in_=x2[:]), reads=[xk2])
                    S.op("act", lambda e, x2=x2: e.activation(out=h2b[:], in_=x2[:], func=AF.Square, scale=1.0 / 32,
                                                              accum_out=ss2[:]), reads=[xk2], writes=["h2b", "ss2"])
                    rsqrt_pool(rstd2[:], ss2[:], "ss2", "rstd2")
                    S.op("dve", lambda e, x2=x2: e.scalar_tensor_tensor(out=h2f[:], in0=x2[:], scalar=rstd2[:], in1=nfw[:],
                                                                        op0=ALU.mult, op1=ALU.mult),
                         reads=[xk2, "rstd2", "nfw"], writes=["h2f"])
                    if s + 2 < NS:
                        load_x(s + 2, b)
                    yield
                    S.op("act", lambda e: e.copy(out=h2b[:], in_=h2f[:]), reads=["h2f"], writes=["h2b"])
                    S.dma("sp", lambda e, t0=t0: e.dma_start(out=h2_d[t0:t0 + 128, :], in_=h2b[:]), reads=["h2b"])
                    S.op("dve", lambda e: e.tensor_tensor(out=hb[:], in0=h2f[:], in1=h2b[:], op=ALU.subtract),
                         reads=["h2f", "h2b"], writes=["hb"])
                    for src, srck, dst, dstk in ((h2b, "h2b", h2Th, "h2Th"), (hb, "hb", h2Tl, "h2Tl")):
                        bt2, bk2 = nextbank()
                        btb = bt2[:].bitcast(BF16).rearrange("p (c t) -> p c t", c=8)
                        for kc in range(8):
                            tr(btb[:, kc, :], src[:, kc * 128:(kc + 1) * 128], identb[:], [srck, "identb"], [bk2])
                        evac(dst[:], btb, [bk2], [dstk])
                        yield
                    bt3, bk3 = nextbank()
                    for kc in range(8):
                        mm(bt3[:, 0:36], h2Th[:, kc, :], wrh[:, kc, :], kc == 0, False, ["h2Th", "wrh"], [bk3])
                        mm(bt3[:, 0:36], h2Tl[:, kc, :], wrh[:, kc, :], False, False, ["h2Tl", "wrh"], [bk3])
                        mm(bt3[:, 0:36], h2Th[:, kc, :], wrl[:, kc, :], False, kc == 7, ["h2Th", "wrl"], [bk3])
                    S.op("dve", lambda e, gb=gb, bt3=bt3: e.tensor_copy(out=lg_all[:, gb, :], in_=bt3[:, 0:36]),
                         reads=[bk3], writes=["lg_all"])
                    yield

            RW = [int(x_) for x_ in os.environ.get("K_RW", "1,1,1").split(",")]

            def run(*gens):
                act_ = [(g_, RW[i_] if len(gens) == 3 else 1) for i_, g_ in enumerate(gens)]
                while act_:
                    for ent in list(act_):
                        for _ in range(ent[1]):
                            try:
                                next(ent[0])
                            except StopIteration:
                                act_.remove(ent)
                                break

            def chain(*gens):
                for g_ in gens:
                    yield from g_

            run(gA(0))
            run(chain(gG(0), gY(0)), gX(0), *([gA(1)] if NS > 1 else []))
            zf = 0
            for s in range(NS):
                for _ in range(2):
                    if zf < NE:
                        zero_fill(zf)
                        zf += 1
                side = [gC(s), gD(s)]
                if s + 2 < NS:
                    side.append(gA(s + 2))
                streams = []
                if s + 1 < NS:
                    streams = [chain(gG(s + 1), gY(s + 1)), gX(s + 1)]
                run(*streams, chain(*side))

        S.barrier()
        if debug:
            S.dma("sp", lambda e: e.dma_start(out=lg_d, in_=lg_all[:]), reads=["lg_all"])

        if not stop_after_phase1:
            with ExitStack() as e2:
                wgt = [sb(e2, "wgt%d" % i, [128, 8, 512], BF16) for i in range(2)]
                wut = [sb(e2, "wut%d" % i, [128, 8, 512], BF16) for i in range(2)]
                wdt = [sb(e2, "wdt%d" % i, [128, 4, D], BF16) for i in range(2)]
                def load_expert(ex_):
                    bi = ex_ % 2
                    S.dma("pool", lambda e: e.dma_start(out=wgt[bi][:],
                                                        in_=wg_d[ex_].rearrange("(kc p) n -> p kc n", p=128)),
                          writes=["wgt%d" % bi])
                    S.dma("pool", lambda e: e.dma_start(out=wut[bi][:],
                                                        in_=wu_d[ex_].rearrange("(kc p) n -> p kc n", p=128)),
                          writes=["wut%d" % bi])
                    S.dma("pool", lambda e: e.dma_start(out=wdt[bi][:],
                                                        in_=wd_d[ex_].rearrange("(kc p) n -> p kc n", p=128)),
                          writes=["wdt%d" % bi])

                load_expert(0)
                load_expert(1)
                brt = sb(e2, "brt", [128, 36], F32)
                eoff = sb(e2, "eoff", [128, NE], F32)
                nlw = sb(e2, "nlw", [128, D], F32)
                ld(brt[:], brt_d.partition_broadcast(128), "brt")
                ld(eoff[:], eoff_d.partition_broadcast(128), "eoff")
                ld(nlw[:], nlw_d.partition_broadcast(128), "nlw")
                gmax = sb(e2, "gmax", [128, NT], F32)
                gsh = sb(e2, "gsh", [128, NT, 4], F32)
                goh = sb(e2, "goh", [128, NT, 4], F32)
                gsum = sb(e2, "gsum", [128, NT], F32)
                gp = sb(e2, "gp", [128, NT], F32)
                elm = sb(e2, "elm", [128, NT, 32], F32)
                pen = sb(e2, "pen", [128, NT, 4], F32)
                m1 = sb(e2, "m1", [128, NT], F32)
                m2 = sb(e2, "m2", [128, NT], F32)
                oh1 = sb(e2, "oh1", [128, NT, 32], F32)
                oh2 = sb(e2, "oh2", [128, NT, 32], F32)
                Mb = sb(e2, "Mb", [128, NT * 32], BF16)
                pos = sb(e2, "pos", [128, NT, 32], F32)
                base = sb(e2, "base", [128, NT, 32], F32)
                tmpe = sb(e2, "tmpe", [128, NT, 32], F32)
                dstf = sb(e2, "dstf", [128, 2, NT], F32)
                dsti = sb(e2, "dsti", [128, 2, NT], I32)
                wts = sb(e2, "wts", [128, 2, NT], F32)
                dlt = sb(e2, "dlt", [128, NT], F32)

                def dv(fn, R, W):
                    S.op("dve", fn, reads=R, writes=W)

                carry = sb(e2, "carry", [128, NE], F32)
                hrow = [sb(e2, "hrow%d" % i, [128, D], BF16) for i in range(4)]
                NH = NT // 2

                def route(hf):
                    J = slice(hf * NH, (hf + 1) * NH)
                    K = lambda n_: n_ + str(hf)
                    lgh = lg_all[:, J, :]
                    dv(lambda e: e.tensor_tensor(out=lgh, in0=lgh, in1=brt[:].unsqueeze(1).to_broadcast([128, NH, 36]),
                                                 op=ALU.add), ["lg_all", "brt"], [K("lg"), "lg_all"])
                    gl = lg_all[:, J, 0:4]
                    el = lg_all[:, J, 4:36]
                    dv(lambda e: e.tensor_reduce(out=gmax[:, J], in_=gl, axis=AX.X, op=ALU.max), [K("lg")], [K("gmax")])
                    dv(lambda e: e.tensor_tensor(out=gsh[:, J, :], in0=gl,
                                                 in1=gmax[:, J].unsqueeze(2).to_broadcast([128, NH, 4]),
                                                 op=ALU.subtract), [K("lg"), K("gmax")], [K("gsh")])
                    dv(lambda e: e.tensor_single_scalar(out=goh[:, J, :], in_=gsh[:, J, :], scalar=0.0, op=ALU.is_ge),
                       [K("gsh")], [K("goh")])
                    S.op("act", lambda e: e.activation(out=gsh[:, J, :], in_=gsh[:, J, :], func=AF.Exp),
                         reads=[K("gsh")], writes=[K("gsh")])
                    dv(lambda e: e.tensor_reduce(out=gsum[:, J], in_=gsh[:, J, :], axis=AX.X, op=ALU.add),
                       [K("gsh")], [K("gsum")])
                    dv(lambda e: e.reciprocal(out=gp[:, J], in_=gsum[:, J]), [K("gsum")], [K("gp")])
                    dv(lambda e: e.tensor_scalar(out=pen[:, J, :], in0=goh[:, J, :], scalar1=1e30, scalar2=-1e30,
                                                 op0=ALU.mult, op1=ALU.add), [K("goh")], [K("pen")])
                    dv(lambda e: e.tensor_tensor(out=elm[:, J, :].rearrange("p j (g k) -> p j g k", g=4),
                                                 in0=el.rearrange("p j (g k) -> p j g k", g=4),
                                                 in1=pen[:, J, :].unsqueeze(3).to_broadcast([128, NH, 4, 8]), op=ALU.add),
                       [K("lg"), K("pen")], [K("elm")])
                    dv(lambda e: e.tensor_reduce(out=m1[:, J], in_=elm[:, J, :], axis=AX.X, op=ALU.max),
                       [K("elm")], [K("m1")])
                    dv(lambda e: e.tensor_tensor(out=oh1[:, J, :], in0=elm[:, J, :],
                                                 in1=m1[:, J].unsqueeze(2).to_broadcast([128, NH, 32]), op=ALU.is_ge),
                       [K("elm"), K("m1")], [K("oh1")])
                    dv(lambda e: e.scalar_tensor_tensor(out=elm[:, J, :], in0=oh1[:, J, :], scalar=-1e30, in1=elm[:, J, :],
                                                        op0=ALU.mult, op1=ALU.add), [K("oh1"), K("elm")], [K("elm")])
                    dv(lambda e: e.tensor_reduce(out=m2[:, J], in_=elm[:, J, :], axis=AX.X, op=ALU.max),
                       [K("elm")], [K("m2")])
                    dv(lambda e: e.tensor_tensor(out=oh2[:, J, :], in0=elm[:, J, :],
                                                 in1=m2[:, J].unsqueeze(2).to_broadcast([128, NH, 32]), op=ALU.is_ge),
                       [K("elm"), K("m2")], [K("oh2")])
                    dv(lambda e: e.tensor_tensor(out=dlt[:, J], in0=m1[:, J], in1=m2[:, J], op=ALU.subtract),
                       [K("m1"), K("m2")], [K("dlt")])
                    S.op("act", lambda e: e.activation(out=dlt[:, J], in_=dlt[:, J], func=AF.Sigmoid),
                         reads=[K("dlt")], writes=[K("dlt")])
                    dv(lambda e: e.tensor_tensor(out=wts[:, 0, J], in0=dlt[:, J], in1=gp[:, J], op=ALU.mult),
                       [K("dlt"), K("gp")], [K("wts")])
                    dv(lambda e: e.tensor_tensor(out=wts[:, 1, J], in0=gp[:, J], in1=wts[:, 0, J], op=ALU.subtract),
                       [K("gp"), K("wts")], [K("wts")])
                    dv(lambda e: e.tensor_tensor(out=Mb[:, hf * 512:(hf + 1) * 512].rearrange("p (j e) -> p j e", j=NH),
                                                 in0=oh1[:, J, :], in1=oh2[:, J, :], op=ALU.add),
                       [K("oh1"), K("oh2")], [K("Mb")])
                    S.op("pe", lambda e: e.matmul(AA[hf][:], lhsT=trisb[:], rhs=Mb[:, hf * 512:(hf + 1) * 512],
                                                  start=True, stop=True), reads=["trisb", K("Mb")], writes=["A%d" % hf])
                    S.op("pe", lambda e: e.matmul(UU[hf][:], lhsT=onesb[:], rhs=Mb[:, hf * 512:(hf + 1) * 512],
                                                  start=True, stop=True), reads=["onesb", K("Mb")], writes=["U%d" % hf])
                    dv(lambda e: e.tensor_copy(out=pos[:, J, :], in_=AA[hf][:].rearrange("p (j e) -> p j e", j=NH)),
                       ["A%d" % hf], [K("pos")])
                    dv(lambda e: e.tensor_copy(out=tmpe[:, J, :], in_=UU[hf][:].rearrange("p (j e) -> p j e", j=NH)),
                       ["U%d" % hf], [K("tmpe")])
                    j0 = hf * NH
                    if hf == 0:
                        dv(lambda e: e.memset(base[:, 0, :], 0.0), [], [K("base")])
                    else:
                        dv(lambda e: e.tensor_copy(out=base[:, j0, :], in_=carry[:]), ["carry"], [K("base")])
                    for j in range(j0 + 1, j0 + NH):
                        dv(lambda e, j=j: e.tensor_tensor(out=base[:, j, :], in0=base[:, j - 1, :], in1=tmpe[:, j - 1, :],
                                                          op=ALU.add), [K("base"), K("tmpe")], [K("base")])
                    if hf == 0:
                        dv(lambda e: e.tensor_tensor(out=carry[:], in0=base[:, NH - 1, :], in1=tmpe[:, NH - 1, :],
                                                     op=ALU.add), [K("base"), K("tmpe")], ["carry"])
                    dv(lambda e: e.tensor_tensor(out=pos[:, J, :], in0=pos[:, J, :], in1=base[:, J, :], op=ALU.add),
                       [K("pos"), K("base")], [K("pos")])
                    dv(lambda e: e.tensor_scalar(out=pos[:, J, :], in0=pos[:, J, :], scalar1=float(CAP - 1), scalar2=None,
                                                 op0=ALU.min), [K("pos")], [K("pos")])
                    dv(lambda e: e.tensor_tensor(out=pos[:, J, :], in0=pos[:, J, :],
                                                 in1=eoff[:].unsqueeze(1).to_broadcast([128, NH, NE]), op=ALU.add),
                       [K("pos"), "eoff"], [K("pos")])
                    for k, oh in enumerate((oh1, oh2)):
                        dv(lambda e, oh=oh: e.tensor_tensor(out=tmpe[:, J, :], in0=pos[:, J, :], in1=oh[:, J, :], op=ALU.mult),
                           [K("pos"), K("oh1"), K("oh2"), K("tmpe"), "carry"], [K("tmpe")])
                        dv(lambda e, k=k: e.tensor_reduce(out=dstf[:, k, J], in_=tmpe[:, J, :], axis=AX.X, op=ALU.add),
                           [K("tmpe")], [K("dstf")])
                    dv(lambda e: e.tensor_copy(out=dsti[:, :, J], in_=dstf[:, :, J]), [K("dstf")], [K("dsti")])
                    for j in range(j0, j0 + NH):
                        hr = hrow[j % 4]
                        hk = "hrow%d" % (j % 4)
                        S.dma("sp", lambda e, j=j, hr=hr: e.dma_start(out=hr[:], in_=h2_d[j * 128:(j + 1) * 128, :]),
                              writes=[hk])
                        for k in range(2):
                            S.dma("pool", lambda e, j=j, k=k, hr=hr: e.indirect_dma_start(
                                out=xs_d, out_offset=bass.IndirectOffsetOnAxis(ap=dsti[:, k, j:j + 1], axis=0),
                                in_=hr[:], in_offset=None), reads=[hk, K("dsti")] + ["xz%d" % z for z in range(NE)])

                route(0)
                route(1)
                if debug:
                    S.dma("sp", lambda e: e.dma_start(out=dst_d, in_=dsti[:]), reads=["dsti0", "dsti1"])
                    S.dma("sp", lambda e: e.dma_start(out=wts_d, in_=wts[:]), reads=["wts0", "wts1"])

                S.barrier()
                xsb = [sb(e2, "xsb%d" % i, [128, NBLK, D], BF16) for i in range(3)]
                xsT = [sb(e2, "xsT%d" % i, [128, 8, CAP], BF16) for i in range(2)]
                sg = [sb(e2, "sg%d" % i, [128, CAP], F32) for i in range(2)]
                aT = [sb(e2, "aT%d" % i, [128, 4, CAP], BF16) for i in range(2)]
                ysb = [sb(e2, "ysb%d" % i, [128, D], F32) for i in range(3)]

                def load_xs(ex_):
                    b3 = ex_ % 3
                    S.dma("sp", lambda e: e.dma_start(
                        out=xsb[b3][:], in_=xs_d[ex_ * CAP:(ex_ + 1) * CAP, :].rearrange("(b p) d -> p b d", p=128)),
                        writes=["xsb%d" % b3])

                def transposes(ex_):
                    b3, b2 = ex_ % 3, ex_ % 2
                    for blk in range(NBLK):
                        for kc in range(8):
                            S.op("pe", lambda e, blk=blk, kc=kc: e.transpose(
                                out=T0[:, kc, :], in_=xsb[b3][:, blk, kc * 128:(kc + 1) * 128], identity=identb[:]),
                                reads=["xsb%d" % b3, "identb"], writes=["T0"])
                        if blk % 2 == 0:
                            S.op("act", lambda e, blk=blk: e.copy(out=xsT[b2][:, :, blk * 128:(blk + 1) * 128], in_=T0[:]),
                                 reads=["T0"], writes=["xsT%d" % b2])
                        else:
                            S.op("dve", lambda e, blk=blk: e.tensor_copy(out=xsT[b2][:, :, blk * 128:(blk + 1) * 128],
                                                                         in_=T0[:]), reads=["T0"], writes=["xsT%d" % b2])

                GU_B = [((A0, "A0"), (A1, "A1")), ((SC, "SC"), (PV0, "PV0"))]
                DN_B = [(U0, "U0"), (U1, "U1"), (PV1, "PV1")]
                dn_i = [0]

                load_xs(0)
                load_xs(1)
                transposes(0)
                yflip = 0
                for ex_ in range(NE):
                    bi = ex_ % 2
                    if ex_ + 1 < NE and ex_ + 1 >= 2:
                        load_expert(ex_ + 1)
                    if ex_ + 2 < NE:
                        load_xs(ex_ + 2)
                    xT = xsT[bi]
                    xk = "xsT%d" % bi
                    aTb = aT[bi]
                    ak_ = "aT%d" % bi
                    for dc in range(4):
                        dsl = slice(dc * 128, (dc + 1) * 128)
                        (bg_, kg_), (bu_, ku_) = GU_B[dc % 2]
                        for kc in range(8):
                            S.op("pe", lambda e, kc=kc, dsl=dsl, bg_=bg_, bi=bi, xT=xT: e.matmul(
                                bg_[:, 0:CAP], lhsT=wgt[bi][:, kc, dsl], rhs=xT[:, kc, :], start=(kc == 0), stop=(kc == 7)),
                                reads=["wgt%d" % bi, xk], writes=[kg_])
                        for kc in range(8):
                            S.op("pe", lambda e, kc=kc, dsl=dsl, bu_=bu_, bi=bi, xT=xT: e.matmul(
                                bu_[:, 0:CAP], lhsT=wut[bi][:, kc, dsl], rhs=xT[:, kc, :], start=(kc == 0), stop=(kc == 7)),
                                reads=["wut%d" % bi, xk], writes=[ku_])
                        sgb = sg[dc % 2]
                        S.op("act", lambda e, sgb=sgb, bg_=bg_: e.activation(out=sgb[:], in_=bg_[:, 0:CAP], func=AF.Silu),
                             reads=[kg_], writes=["sg%d" % (dc % 2)])
                        dv(lambda e, dc=dc, sgb=sgb, bu_=bu_, aTb=aTb: e.tensor_tensor(out=aTb[:, dc, :], in0=bu_[:, 0:CAP], in1=sgb[:],
                                                                             op=ALU.mult),
                           [ku_, "sg%d" % (dc % 2)], [ak_])
                    if ex_ + 1 < NE:
                        transposes(ex_ + 1)
                    for blk in range(NBLK):
                        yb = ysb[yflip]
                        yk = "ysb%d" % yflip
                        yflip = (yflip + 1) % 3
                        for half in range(2):
                            bd_, kd_ = DN_B[dn_i[0] % 3]
                            dn_i[0] += 1
                            for dc in range(4):
                                S.op("pe", lambda e, blk=blk, half=half, dc=dc, bd_=bd_, bi=bi, aTb=aTb: e.matmul(
                                    bd_[:], lhsT=aTb[:, dc, blk * 128:(blk + 1) * 128],
                                    rhs=wdt[bi][:, dc, half * 512:(half + 1) * 512], start=(dc == 0), stop=(dc == 3)),
                                    reads=[ak_, "wdt%d" % bi], writes=[kd_])
                            if half == 0:
                                S.op("act", lambda e, yb=yb, bd_=bd_: e.copy(out=yb[:, 0:512], in_=bd_[:]), reads=[kd_], writes=[yk])
                            else:
                                dv(lambda e, yb=yb, bd_=bd_: e.tensor_copy(out=yb[:, 512:1024], in_=bd_[:]), [kd_], [yk])
                        r0 = ex_ * CAP + blk * 128
                        S.dma("sp", lambda e, r0=r0, yb=yb: e.dma_start(out=ys_d[r0:r0 + 128, :], in_=yb[:]),
                              reads=[yk])

                S.barrier()
                NB = 4
                y0 = [sb(e2, "y0_%d" % i, [128, D], F32) for i in range(NB)]
                y1 = [sb(e2, "y1_%d" % i, [128, D], F32) for i in range(NB)]
                xr = [sb(e2, "xr%d" % i, [128, D], F32) for i in range(NB)]
                jk2 = sb(e2, "jk2", [128, D], BF16)
                ss3 = [sb(e2, "ss3_%d" % i, [128, 1], F32) for i in range(2)]
                rs3 = [sb(e2, "rs3_%d" % i, [128, 1], F32) for i in range(2)]
                def comb_loads(j):
                    bi = j % NB
                    S.dma("sp", lambda e: e.dma_start(out=xr[bi][:], in_=x2_d[j * 128:(j + 1) * 128, :]),
                          writes=["xr%d" % bi])
                    for k, yy in enumerate((y0, y1)):
                        S.dma("pool", lambda e, k=k, yy=yy: e.indirect_dma_start(
                            out=yy[bi][:], out_offset=None, in_=ys_d,
                            in_offset=bass.IndirectOffsetOnAxis(ap=dsti[:, k, j:j + 1], axis=0)),
                            reads=["dsti0", "dsti1"], writes=["y%d_%d" % (k, bi)])

                for j in range(NB - 1):
                    comb_loads(j)
                for j in range(NT):
                    bi = j % NB
                    b2 = j % 2
                    if j + NB - 1 < NT:
                        comb_loads(j + NB - 1)
                    dv(lambda e, j=j, bi=bi: e.scalar_tensor_tensor(out=xr[bi][:], in0=y0[bi][:], scalar=wts[:, 0, j:j + 1],
                                                                    in1=xr[bi][:], op0=ALU.mult, op1=ALU.add),
                       ["y0_%d" % bi, "wts0", "wts1", "xr%d" % bi], ["xr%d" % bi])
                    dv(lambda e, j=j, bi=bi: e.scalar_tensor_tensor(out=xr[bi][:], in0=y1[bi][:], scalar=wts[:, 1, j:j + 1],
                                                                    in1=xr[bi][:], op0=ALU.mult, op1=ALU.add),
                       ["y1_%d" % bi, "wts0", "wts1", "xr%d" % bi], ["xr%d" % bi])
                    S.op("act", lambda e, bi=bi, b2=b2: e.activation(out=jk2[:], in_=xr[bi][:], func=AF.Square,
                                                                     scale=1.0 / 32, accum_out=ss3[b2][:]),
                         reads=["xr%d" % bi], writes=["jk2", "ss3_%d" % b2])
                    S.op("act", lambda e, b2=b2: e.activation(out=rs3[b2][:], in_=ss3[b2][:], func=AF.Sqrt, bias=epsb[:],
                                                              scale=1.0),
                         reads=["ss3_%d" % b2, "epsb"], writes=["rs3_%d" % b2])
                    dv(lambda e, b2=b2: e.reciprocal(out=rs3[b2][:], in_=rs3[b2][:]), ["rs3_%d" % b2], ["rs3_%d" % b2])
                    dv(lambda e, bi=bi, b2=b2: e.scalar_tensor_tensor(out=y0[bi][:], in0=xr[bi][:], scalar=rs3[b2][:],
                                                                      in1=nlw[:], op0=ALU.mult, op1=ALU.mult),
                       ["xr%d" % bi, "rs3_%d" % b2, "nlw"], ["y0_%d" % bi])
                    S.dma("sp", lambda e, j=j, bi=bi: e.dma_start(out=out_d[j * 128:(j + 1) * 128, :], in_=y0[bi][:]),
                          reads=["y0_%d" % bi])
        S.barrier()
        S.emit()
        print("total ops", S.nops, {k: S.cnt[k] for k in S.ENG})
    return nc


def make_consts():
    bf = ml_dtypes.bfloat16
    i = np.arange(128)
    s_le_t = (i[:, None] <= i[None, :])
    maskb = np.zeros((128, 3, 128), np.float32)
    maskb[:, 0, :] = np.where(i[:, None] > i[None, :], 0.0, -30000.0)
    maskb[:, 1, :] = np.where(i[:, None] <= i[None, :], 0.0, -30000.0)
    maskb[:, 2, :] = -30000.0
    return {
        "ident_bf": np.eye(128, dtype=np.float32).astype(bf),
        "ident_f": np.eye(128, dtype=np.float32),
        "maskb": maskb.astype(bf),
        "cmask8": (0.03125 * s_le_t).astype(np.float32),
        "tri_f": s_le_t.astype(np.float32),
        "ones_f": np.ones((128, 128), np.float32),
        "tris_bf": (i[:, None] < i[None, :]).astype(np.float32).astype(bf),
        "ones_bf": np.ones((128, 128), np.float32).astype(bf),
        "eoff": (np.arange(NE, dtype=np.float32) * CAP)[None, :],
    }


def make_in_maps(inputs, cores):
    f = lambda a: np.ascontiguousarray(np.asarray(a, dtype=np.float32))
    x = f(inputs["x"])
    conv_w = f(inputs["conv_w"])[0]
    conv_b = f(inputs["conv_b"])[0]
    shared = {
        "w_in": f(inputs["w_in"])[0],
        "w_gab": np.ascontiguousarray(
            f(inputs["w_in"])[0][:, C_GA:C_GA + 2048].reshape(8, 128, 16, 128).transpose(2, 1, 0, 3)),
        "w_attn_o": f(inputs["w_attn_o"])[0],
        "w_mlstm_o": f(inputs["w_mlstm_o"])[0],
        "w_out": f(inputs["w_out"])[0],
        "w_rt": np.ascontiguousarray(np.concatenate([f(inputs["w_group"])[0], f(inputs["w_router"])[0]], axis=1)),
        "w_gate": f(inputs["w_gate"])[0],
        "w_up": f(inputs["w_up"])[0],
        "w_down": f(inputs["w_down"])[0],
        "norm_mix_w": f(inputs["norm_mix_w"]).reshape(1, D),
        "norm_ffn_w": f(inputs["norm_ffn_w"]).reshape(1, D),
        "norm_final_w": f(inputs["norm_final_w"]).reshape(1, D),
        "mlstm_norm_w": f(inputs["mlstm_norm_w"]).reshape(1, 512),
        "conv_wt": np.ascontiguousarray(conv_w.reshape(4, 4, 128).transpose(2, 1, 0)),
        "conv_bt": np.ascontiguousarray(conv_b.reshape(4, 128).T),
        "b_gates": np.ascontiguousarray(np.concatenate([f(inputs["b_igate"])[0], f(inputs["b_fgate"])[0]])[None, :]),
        "attn_sinks": f(inputs["attn_sinks"]).reshape(1, 8),
        "b_rt": np.ascontiguousarray(np.concatenate([f(inputs["b_group"])[0], f(inputs["b_router"])[0]])[None, :]),
    }
    shared.update(make_consts())
    return [dict(shared, x=np.ascontiguousarray(x[c])) for c in cores]


def kernel(**inputs):
    nc = build_program()
    in_maps = make_in_maps(inputs, range(8))
    res = run_bass_kernel_spmd(nc, in_maps, core_ids=list(range(8)))
    return np.stack([np.asarray(r["out"], dtype=np.float32) for r in res.results], axis=0)
```
